# Optimizing a Trainium2 kernel written in Bass

```python
import math
import jax
import jax.numpy as jnp
from jax import lax
import numpy as np

D_MODEL = 2048
BATCH = 2
SEQ = 4096
DEPTH = 2

CHUNK = 64
N_EVEN = (DEPTH + 1) // 2
N_ODD = DEPTH // 2
ALPHA = (2.0 * DEPTH) ** 0.25
BETA = (8.0 * DEPTH) ** -0.25
LN_EPS = 1e-5
NEG = -1e30

A_WIDTH = D_MODEL // 2
A_BLOCK = 128
A_GROUP_CH = 128
A_GROUPS = A_WIDTH // A_GROUP_CH
B_WIDTH = D_MODEL - A_WIDTH
DIFF_HEAD_DIM = 64
DIFF_V_DIM = 2 * DIFF_HEAD_DIM
DIFF_HEADS = B_WIDTH // DIFF_V_DIM
Q_BLOCK = 128
EVEN_IN = 2 * A_WIDTH + 3 * B_WIDTH
POOL_WINDOWS = (2, 4, 8, 16)
POOL_GROUP_CH = D_MODEL // len(POOL_WINDOWS)
N_MEM = 256
XA_HEADS = 4
XA_HEAD_DIM = 128
XA_WIDTH = XA_HEADS * XA_HEAD_DIM
N_EXPERTS = 32
TOP_K = 4
D_FF = D_MODEL
SWIGLU_LIMIT = 7.0
SWIGLU_ALPHA = 1.702
MOE_BLOCK = 256

kernel_name = 'hybrid_gmlp_diffattn_pool_moe_deepnorm'


def _layernorm(x, g, b):
    xf = x.astype(jnp.float32)
    mu = xf.mean(-1, keepdims=True)
    var = jnp.square(xf - mu).mean(-1, keepdims=True)
    return ((xf - mu) * lax.rsqrt(var + LN_EPS) * g + b).astype(x.dtype)


def _rmsnorm(x, g):
    xf = x.astype(jnp.float32)
    return (xf * lax.rsqrt(jnp.square(xf).mean(-1, keepdims=True) + LN_EPS) * g).astype(x.dtype)


def _alibi_slopes(n):
    return jnp.asarray(2.0 ** (-8.0 * np.arange(1, n + 1) / n), dtype=jnp.float32)


def _spatial_gating(u, v, ln_g, ln_b, w_s, b_s):
    bsz, seq, _ = v.shape
    v = _layernorm(v, ln_g, ln_b)
    vr = v.reshape(bsz, seq // A_BLOCK, A_BLOCK, A_GROUPS, A_GROUP_CH)
    cid = jnp.arange(A_BLOCK) // CHUNK
    mask = cid[None, :] <= cid[:, None]
    w = jnp.where(mask[None], w_s, 0.0)
    z = jnp.einsum('gts,bnsgc->bntgc', w, vr) + b_s.T[None, None, :, :, None]
    return u * z.reshape(bsz, seq, A_WIDTH)


def _diff_attention(q, k, v, lam, subln_g, lambda_init):
    bsz, seq, _ = q.shape
    nb = seq // Q_BLOCK
    q = q.reshape(bsz, nb, Q_BLOCK, DIFF_HEADS, 2, DIFF_HEAD_DIM).transpose(1, 0, 3, 4, 2, 5)
    k = k.reshape(bsz, seq, DIFF_HEADS, 2, DIFF_HEAD_DIM).transpose(0, 2, 3, 1, 4)
    v = v.reshape(bsz, seq, DIFF_HEADS, DIFF_V_DIM).transpose(0, 2, 1, 3)
    slopes = _alibi_slopes(DIFF_HEADS)
    k_pos = jnp.arange(seq)
    scale = DIFF_HEAD_DIM ** -0.5

    def block(args):
        qb, i = args
        q_pos = i * Q_BLOCK + jnp.arange(Q_BLOCK)
        s = jnp.einsum('bhmqd,bhmkd->bhmqk', qb, k).astype(jnp.float32) * scale
        dist = jnp.abs(q_pos[:, None] - k_pos[None, :]).astype(jnp.float32)
        allowed = (k_pos[None, :] // CHUNK) <= (q_pos[:, None] // CHUNK)
        s = jnp.where(allowed, s - slopes[:, None, None, None] * dist, NEG)
        p = jax.nn.softmax(s, axis=-1)
        a = p[:, :, 0] - lam * p[:, :, 1]
        return jnp.einsum('bhqk,bhkd->bhqd', a.astype(v.dtype), v)

    o = lax.map(block, (q, jnp.arange(nb)))
    o = o.transpose(1, 0, 3, 2, 4).reshape(bsz, seq, DIFF_HEADS, DIFF_V_DIM)
    o = _rmsnorm(o, subln_g) * (1.0 - lambda_init)
    return o.reshape(bsz, seq, B_WIDTH)


def _even_mixer(x, w_in, ln_v_g, ln_v_b, w_s, b_s, lq1, lk1, lq2, lk2, subln_g, w_out, lambda_init):
    h = x @ w_in
    uv, q, k, v = jnp.split(h, [2 * A_WIDTH, 2 * A_WIDTH + B_WIDTH, 2 * A_WIDTH + 2 * B_WIDTH], axis=-1)
    u, gv = jnp.split(jax.nn.gelu(uv, approximate=False), 2, axis=-1)
    a_out = _spatial_gating(u, gv, ln_v_g, ln_v_b, w_s, b_s)
    lam = (jnp.exp(jnp.sum(lq1.astype(jnp.float32) * lk1.astype(jnp.float32)))
           - jnp.exp(jnp.sum(lq2.astype(jnp.float32) * lk2.astype(jnp.float32))) + lambda_init)
    b_out = _diff_attention(q, k, v, lam, subln_g, lambda_init)
    return jnp.concatenate([a_out, b_out], axis=-1) @ w_out


def _trailing_mean_minus_self(h, window):
    seq = h.shape[1]
    hf = h.astype(jnp.float32)
    c = jnp.cumsum(hf, axis=1)
    lagged = jnp.pad(c, ((0, 0), (window, 0), (0, 0)))[:, :seq]
    count = jnp.minimum(jnp.arange(seq) + 1, window).astype(jnp.float32)[None, :, None]
    return ((c - lagged) / count - hf).astype(h.dtype)


def _odd_mixer(x, w_in, w_grp, scale, w_out):
    h = x @ w_in
    groups = jnp.split(h, len(POOL_WINDOWS), axis=-1)
    y = jnp.concatenate([_trailing_mean_minus_self(g, w) @ w_grp[j]
                         for j, (g, w) in enumerate(zip(groups, POOL_WINDOWS))], axis=-1)
    return (y * scale) @ w_out


def _cross_attention(x, mem, w_q, w_k, w_v, w_o):
    bsz, seq, _ = x.shape
    n_mem = mem.shape[1]
    q = (x @ w_q).reshape(bsz, seq, XA_HEADS, XA_HEAD_DIM)
    k = (mem @ w_k).reshape(bsz, n_mem, XA_HEADS, XA_HEAD_DIM)
    v = (mem @ w_v).reshape(bsz, n_mem, XA_HEADS, XA_HEAD_DIM)
    s = jnp.einsum('bqhd,bkhd->bhqk', q, k).astype(jnp.float32) * (XA_HEAD_DIM ** -0.5)
    p = jax.nn.softmax(s, axis=-1)
    o = jnp.einsum('bhqk,bkhd->bqhd', p.astype(v.dtype), v).reshape(bsz, seq, XA_WIDTH)
    return o @ w_o


def _moe(x, w_router, b_router, w_gu, b_gu, w_down, b_down):
    bsz, seq, d = x.shape
    xt = x.reshape(-1, d)
    n = xt.shape[0]
    logits = (xt @ w_router + b_router).astype(jnp.float32)
    top_val, top_idx = lax.top_k(logits, TOP_K)
    gates = jax.nn.softmax(top_val, axis=-1)
    n_assign = n * TOP_K
    e_flat = top_idx.reshape(-1)
    tok_flat = jnp.arange(n_assign) // TOP_K
    g_flat = gates.reshape(-1)
    order = jnp.argsort(e_flat)
    e_s, tok_s, g_s = e_flat[order], tok_flat[order], g_flat[order]
    counts = jnp.bincount(e_flat, length=N_EXPERTS)
    starts = jnp.cumsum(counts) - counts
    padded = (counts + MOE_BLOCK - 1) // MOE_BLOCK * MOE_BLOCK
    p_ends = jnp.cumsum(padded)
    p_starts = p_ends - padded
    dest = p_starts[e_s] + (jnp.arange(n_assign) - starts[e_s])
    n_rows = -(-n_assign // MOE_BLOCK) * MOE_BLOCK + N_EXPERTS * MOE_BLOCK
    n_blocks = n_rows // MOE_BLOCK
    block_e = jnp.minimum(jnp.searchsorted(p_ends, jnp.arange(n_blocks) * MOE_BLOCK, side='right'), N_EXPERTS - 1)
    x_rows = jnp.zeros((n_rows, d), x.dtype).at[dest].set(xt[tok_s])

    def expert_block(args):
        xb, e = args
        h = xb @ w_gu[e] + b_gu[e]
        glu, lin = jnp.split(h, 2, axis=-1)
        glu = jnp.minimum(glu, SWIGLU_LIMIT)
        lin = jnp.clip(lin, -SWIGLU_LIMIT, SWIGLU_LIMIT)
        act = glu * jax.nn.sigmoid(SWIGLU_ALPHA * glu) * (lin + 1.0)
        return act @ w_down[e] + b_down[e]

    y_rows = lax.map(expert_block, (x_rows.reshape(n_blocks, MOE_BLOCK, d), block_e)).reshape(n_rows, d)
    y = jax.ops.segment_sum(y_rows[dest] * g_s[:, None].astype(x.dtype), tok_s, num_segments=n)
    return y.reshape(bsz, seq, d)


def _init(key, shape, std):
    a = std * math.sqrt(3.0)
    return jax.random.uniform(key, shape, jnp.float32, -a, a)


def setup_inputs(seed: int = 0) -> dict:
    key = jax.random.key(seed)
    ks = jax.random.split(key, 32)
    d = D_MODEL
    f32 = jnp.float32

    def noise(k, shape, s):
        return s * jax.random.normal(k, shape, f32)

    return {
        'x': jax.random.normal(ks[0], (BATCH, SEQ, d), f32),
        'mem': jax.random.normal(ks[1], (BATCH, N_MEM, d), f32),
        'even_w_in': _init(ks[2], (N_EVEN, d, EVEN_IN), d ** -0.5),
        'even_ln_v_g': 1.0 + noise(ks[3], (N_EVEN, A_WIDTH), 0.02),
        'even_ln_v_b': noise(ks[4], (N_EVEN, A_WIDTH), 0.02),
        'even_w_s': _init(ks[5], (N_EVEN, A_GROUPS, A_BLOCK, A_BLOCK), A_BLOCK ** -0.5),
        'even_b_s': 1.0 + noise(ks[6], (N_EVEN, A_GROUPS, A_BLOCK), 0.02),
        'even_lam_q1': noise(ks[7], (N_EVEN, DIFF_HEAD_DIM), 0.1),
        'even_lam_k1': noise(ks[8], (N_EVEN, DIFF_HEAD_DIM), 0.1),
        'even_lam_q2': noise(ks[9], (N_EVEN, DIFF_HEAD_DIM), 0.1),
        'even_lam_k2': noise(ks[10], (N_EVEN, DIFF_HEAD_DIM), 0.1),
        'even_subln_g': 1.0 + noise(ks[11], (N_EVEN, DIFF_V_DIM), 0.02),
        'even_w_out': _init(ks[12], (N_EVEN, d, d), BETA * d ** -0.5),
        'odd_w_in': _init(ks[13], (N_ODD, d, d), d ** -0.5),
        'odd_w_grp': _init(ks[14], (N_ODD, len(POOL_WINDOWS), POOL_GROUP_CH, POOL_GROUP_CH), POOL_GROUP_CH ** -0.5),
        'odd_scale': 1.0 + noise(ks[15], (N_ODD, d), 0.02),
        'odd_w_out': _init(ks[16], (N_ODD, d, d), BETA * d ** -0.5),
        'xa_w_q': _init(ks[17], (DEPTH, d, XA_WIDTH), d ** -0.5),
        'xa_w_k': _init(ks[18], (DEPTH, d, XA_WIDTH), d ** -0.5),
        'xa_w_v': _init(ks[19], (DEPTH, d, XA_WIDTH), d ** -0.5),
        'xa_w_o': _init(ks[20], (DEPTH, XA_WIDTH, d), BETA * XA_WIDTH ** -0.5),
        'moe_w_router': _init(ks[21], (DEPTH, d, N_EXPERTS), d ** -0.5),
        'moe_b_router': noise(ks[22], (DEPTH, N_EXPERTS), 0.01),
        'moe_w_gu': _init(ks[23], (DEPTH, N_EXPERTS, d, 2 * D_FF), d ** -0.5),
        'moe_b_gu': noise(ks[24], (DEPTH, N_EXPERTS, 2 * D_FF), 0.02),
        'moe_w_down': _init(ks[25], (DEPTH, N_EXPERTS, D_FF, d), BETA * D_FF ** -0.5),
        'moe_b_down': noise(ks[26], (DEPTH, N_EXPERTS, d), 0.02),
        'ln_g': 1.0 + noise(ks[27], (DEPTH, 3, d), 0.02),
        'ln_b': noise(ks[28], (DEPTH, 3, d), 0.02),
    }


def reference(x, mem, even_w_in, even_ln_v_g, even_ln_v_b, even_w_s, even_b_s,
              even_lam_q1, even_lam_k1, even_lam_q2, even_lam_k2, even_subln_g, even_w_out,
              odd_w_in, odd_w_grp, odd_scale, odd_w_out,
              xa_w_q, xa_w_k, xa_w_v, xa_w_o,
              moe_w_router, moe_b_router, moe_w_gu, moe_b_gu, moe_w_down, moe_b_down,
              ln_g, ln_b):
    for l in range(DEPTH):
        i = l // 2
        if l % 2 == 0:
            lambda_init = 0.8 - 0.6 * math.exp(-0.3 * l)
            m = _even_mixer(x, even_w_in[i], even_ln_v_g[i], even_ln_v_b[i], even_w_s[i], even_b_s[i],
                            even_lam_q1[i], even_lam_k1[i], even_lam_q2[i], even_lam_k2[i],
                            even_subln_g[i], even_w_out[i], lambda_init)
        else:
            m = _odd_mixer(x, odd_w_in[i], odd_w_grp[i], odd_scale[i], odd_w_out[i])
        x = _layernorm(ALPHA * x + m, ln_g[l, 0], ln_b[l, 0])
        c = _cross_attention(x, mem, xa_w_q[l], xa_w_k[l], xa_w_v[l], xa_w_o[l])
        x = _layernorm(ALPHA * x + c, ln_g[l, 1], ln_b[l, 1])
        f = _moe(x, moe_w_router[l], moe_b_router[l], moe_w_gu[l], moe_b_gu[l], moe_w_down[l], moe_b_down[l])
        x = _layernorm(ALPHA * x + f, ln_g[l, 2], ln_b[l, 2])
    return x
```

```python
import contextlib
import math
import numpy as np
import concourse.bass as bass
import concourse.mybir as mybir
from concourse.bass_utils import run_bass_kernel_spmd

F32 = mybir.dt.float32
BF16 = mybir.dt.bfloat16
AF = mybir.ActivationFunctionType
ALU = mybir.AluOpType

D = 2048
SEQ = 4096
NCORES = 8
T = 1024
NT = 8
NT9 = 9
T9 = NT9 * 128
ALPHA = (2.0 * 2) ** 0.25
LN_EPS = 1e-5
NEGBIG = -1.0e9
NE = 32
CAP = 256
SW_LIMIT = 7.0
SW_ALPHA = 1.702
RING = 3
FLEXW = 11008


class Buf:
    __slots__ = ("name", "w", "r")

    def __init__(self, name):
        self.name = name
        self.w = None
        self.r = {}


class DSem:
    __slots__ = ("key", "sem", "cnt")

    def __init__(self, key, sem):
        self.key = key
        self.sem = sem
        self.cnt = 0


class KB:
    def __init__(self, nc, es):
        self.nc = nc
        self.es = es
        self.engs = {"pe": nc.tensor, "dve": nc.vector, "act": nc.scalar,
                     "pool": nc.gpsimd, "sp": nc.sync}
        self.sems = {}
        self.cnt = {}
        self.known = {}
        for e in self.engs:
            self.sems[e] = es.enter_context(nc.semaphore("s_" + e))
            self.cnt[e] = 0
            self.known[e] = {}
        self.dsems = []

    def dsem(self, name):
        nm = "d_%s_%d" % (name, len(self.dsems))
        d = DSem(nm, self.es.enter_context(self.nc.semaphore(nm)))
        self.sems[d.key] = d.sem
        self.dsems.append(d)
        return d

    def _wait(self, eng, toks):
        kn = self.known[eng]
        need = {}
        for t in toks:
            if t is None:
                continue
            k, v = t
            if eng == "pe" and k == "pe":
                continue
            if kn.get(k, 0) < v and need.get(k, 0) < v:
                need[k] = v
        for k, v in need.items():
            self.engs[eng].wait_ge(self.sems[k], v)
            kn[k] = v

    @staticmethod
    def _deps(reads, writes):
        toks = []
        for b in reads:
            toks.append(b.w)
        for b in writes:
            toks.append(b.w)
            toks.extend(b.r.items())
        return toks

    def op(self, eng, fn, reads=(), writes=(), sig=True):
        self._wait(eng, self._deps(reads, writes))
        inst = fn(self.engs[eng])
        if not sig:
            return inst
        self.cnt[eng] += 1
        inst.then_inc(self.sems[eng], 1)
        v = self.cnt[eng]
        for b in reads:
            b.r[eng] = v
        for b in writes:
            b.w = (eng, v)
            b.r = {}
        return inst

    def dma(self, q, out, in_, dsem, reads=(), writes=()):
        self._wait(q, self._deps(reads, writes))
        inst = self.engs[q].dma_start(out=out, in_=in_)
        dsem.cnt += 16
        inst.then_inc(dsem.sem, 16)
        for b in reads:
            b.r[dsem.key] = dsem.cnt
        for b in writes:
            b.w = (dsem.key, dsem.cnt)
            b.r = {}
        return inst

    def _all(self):
        toks = [(e, self.cnt[e]) for e in self.engs if self.cnt[e] > 0]
        toks += [(d.key, d.cnt) for d in self.dsems if d.cnt > 0]
        return toks

    def barrier(self):
        toks = self._all()
        for e in self.engs:
            self._wait(e, toks)

    def finish(self):
        self._wait("sp", self._all())


class PsumPool:
    def __init__(self, kb, n=8):
        self.banks = []
        for i in range(n):
            t = kb.es.enter_context(kb.nc.psum_tensor("psb%d" % i, [128, 512], F32))
            self.banks.append((t, Buf("psb%d" % i)))
        self.pinned = set()
        self.i = 0

    def get(self, pin=False):
        n = len(self.banks)
        for _ in range(n):
            j = self.i
            self.i = (self.i + 1) % n
            if j not in self.pinned:
                if pin:
                    self.pinned.add(j)
                t, b = self.banks[j]
                return j, t, b
        raise RuntimeError("no free psum bank")

    def unpin(self, j):
        self.pinned.discard(j)


def view(base, off, n, dt=F32, pat=None, **kw):
    ap = base[:, off:off + n]
    if dt != F32:
        ap = ap.bitcast(dt)
    if pat is not None:
        ap = ap.rearrange(pat, **kw)
    return ap


class Prog:
    def __init__(self, dbg=None):
        self.dbg = dbg or ()
        self.nc = bass.Bass("TRN2", target_bir_lowering=False)
        self.inputs = {}

    def din(self, name, shape):
        ap = self.nc.dram_tensor(name, list(shape), F32, kind="ExternalInput").ap()
        self.inputs[name] = ap
        return ap

    def mark(self, name):
        if not hasattr(self, "marks"):
            self.marks = []
        self.marks.append((name, dict(self.kb.cnt)))

    def set_tokens(self, tiles, ranges):
        self.tiles = list(tiles)
        self.col0 = {t: 128 * i for i, t in enumerate(self.tiles)}
        self.ntok = 128 * len(self.tiles)
        self.ranges = list(ranges)

    def build(self):
        nc = self.nc
        with contextlib.ExitStack() as es:
            self.es = es
            kb = self.kb = KB(nc, es)
            self.pp = PsumPool(kb)
            self._declare_io()
            self._alloc()
            self._consts()
            self.L = 0
            self.set_tokens(range(NT9), [(0, 128), (128, 512), (640, 512)])
            self._mixer_even()
            self.mark("mixer0")
            self._wout_stage(self.i_wout0)
            self._layernorm(0)
            self.mark("wout_ln0")
            self._xattn()
            self._layernorm(1)
            self.mark("xattn0")
            self._moe()
            self.mark("moe0")
            self._layernorm(2)
            self.mark("ln0_2")
            self.L = 1
            self._mixer_odd()
            self.mark("mixer1")
            self.set_tokens(range(1, NT9), [(0, 512), (512, 512)])
            self._wout_stage(self.i_wout1)
            self._layernorm(0)
            self.mark("wout_ln1")
            self._xattn()
            self._layernorm(1)
            self.mark("xattn1")
            self._moe()
            self.mark("moe1")
            self._layernorm(2, make_xT=False)
            self._store_out()
            kb.finish()
        return nc

    def _declare_io(self):
        d = self.din
        self.i_xrel = d("x_rel", [SEQ, D])
        self.i_att = d("att_tabs", [5, 128, 512])
        self.i_abias = d("att_bias", [128, 768])
        self.i_hv = d("halo_valid", [128, 1])
        self.i_win0 = d("even_w_in", [D, 5120])
        self.i_lnvg = d("even_ln_v_g", [1024])
        self.i_lnvb = d("even_ln_v_b", [1024])
        self.i_wsT = d("w_sT", [128, 8 * 128])
        self.i_bs = d("even_b_s", [1024])
        self.i_lam = d("lam_vecs", [4 * 64])
        self.i_subg = d("even_subln_g", [128, 1])
        self.i_wout0 = d("even_w_out", [D, D])
        self.i_win1 = d("odd_w_in", [D, D])
        self.i_wgrp = d("odd_w_grp", [D, 512])
        self.i_scale = d("odd_scaleT", [128, 16])
        self.i_wout1 = d("odd_w_out", [D, D])
        self.i_ptab = d("pool_tabs", [128, 4 * 4 * 128])
        self.i_pinv = d("pool_invc", [128, 4 * 128])
        self.i_mem = d("mem", [256, D])
        self.i_wq = d("xa_w_q", [2, D, 512])
        self.i_wk = d("xa_w_k", [2, D, 512])
        self.i_wv = d("xa_w_v", [2, D, 512])
        self.i_wo = d("xa_w_o", [2, 512, D])
        self.i_wr = d("moe_w_router", [2, D, NE])
        self.i_br = d("moe_b_router", [2, NE])
        self.i_wgu = d("moe_w_gu", [2, NE, D, 2 * D])
        self.i_bgu = d("moe_b_guT", [2, 128, NE * 32])
        self.i_wd = d("moe_w_down", [2, NE, D, D])
        self.i_bd = d("moe_b_down", [2, NE, D])
        self.i_lng = d("ln_g", [2, 3, D])
        self.i_lnb = d("ln_b", [2, 3, D])
        self.o_y = self.nc.dram_tensor("y", [T, D], F32, kind="ExternalOutput").ap()
        self.kT_scr = self.nc.dram_tensor("kT_scr", [8, 128, SEQ], BF16, kind="Internal").ap()
        self.v_scr = self.nc.dram_tensor("v_scr", [8, 128, 32, 128], BF16, kind="Internal").ap()
        self.dbg_out = {}
        for name, shape in self.dbg:
            self.dbg_out[name] = self.nc.dram_tensor("dbg_" + name, list(shape), F32, kind="ExternalOutput").ap()

    def _alloc(self):
        nc, es, kb = self.nc, self.es, self.kb
        sb = lambda name, shape, dt: es.enter_context(nc.sbuf_tensor(name, shape, dt))
        self.XRES = sb("XRES", [128, NT9 * D], F32)
        self.CAT = sb("CAT", [128, 9216], F32)
        self.FLEX = sb("FLEX", [128, FLEXW], F32)
        self.RINGT = sb("RINGT", [128, RING * 4096], F32)
        self.ring_bufs = [Buf("ring%d" % i) for i in range(RING)]
        self.ring_ds = [kb.dsem("ring%d" % i) for i in range(RING)]
        self.ring_i = 0
        self.ident = sb("ident", [128, 128], F32)
        self.ones_bf = sb("ones_bf", [128, 128], BF16)
        self.ustrict = sb("ustrict", [128, 128], BF16)
        self.iota256 = sb("iota256", [128, 256], F32)
        self.pcol = sb("pcol", [128, 2], F32)
        self.kidx = sb("kidx", [32, 128], F32)
        self.epsc = sb("epsc", [128, 1], F32)
        self.brB = sb("brB", [128, 2 * NE], F32)
        self.lg = sb("lg", [128, NT9, NE], F32)
        self.maskt = sb("maskt", [128, NT9, NE], F32)
        self.mask_bf = sb("mask_bf", [128, NT9, NE], BF16)
        self.gate = sb("gate", [128, NT9, NE], F32)
        self.rankm = sb("rankm", [128, NT9, NE], F32)
        self.ex = sb("ex", [128, NE], F32)
        self.top8 = sb("top8", [128, 8], F32)
        self.sc = sb("sc", [128, 16], F32)
        self.stats = sb("stats", [128, 4, 6], F32)
        self.mv = sb("mv", [128, 2], F32)
        self.lamc = sb("lamc", [128, 8], F32)
        self.subg = sb("subg", [128, 1], F32)
        self.hv = sb("hv", [128, 1], F32)
        self.bgt = sb("bgt", [128, 2, 32], F32)
        self.B_const = Buf("const")
        self.B_xres = [Buf("xres%d" % i) for i in range(NT9)]
        self.B_small = Buf("small")
        self.d_misc = kb.dsem("misc")
        self.d_io = kb.dsem("io")
        self.evac_i = 0

    def xres(self, tt):
        return self.XRES[:, tt * D:(tt + 1) * D]

    def catT(self):
        return view(self.CAT, 0, self.ntok * 8, BF16, "p (c n) -> p c n", c=16)

    def xT(self):
        return view(self.FLEX, 0, self.ntok * 8, BF16, "p (c n) -> p c n", c=16)

    def wtile(self, src_ap):
        kb = self.kb
        i = self.ring_i
        self.ring_i = (i + 1) % RING
        a, b = src_ap.shape[1], src_ap.shape[2]
        dst = view(self.RINGT, i * 4096, (a * b) // 2, BF16, "p (a b) -> p a b", a=a)
        kb.dma("pool", dst, src_ap, self.ring_ds[i], writes=[self.ring_bufs[i]])
        return dst, self.ring_bufs[i]

    def ring_f32(self, n):
        i = self.ring_i
        self.ring_i = (i + 1) % RING
        return view(self.RINGT, i * 4096, n), self.ring_bufs[i], self.ring_ds[i]

    def wcols(self, w_ap, c0, n=512):
        return w_ap[:, c0:c0 + n].rearrange("(c p) n -> p c n", p=128)

    def evac(self, out, in_, reads, writes, eng=None):
        kb = self.kb
        if eng is None:
            eng = "act" if (self.evac_i % 2 == 0) else "dve"
            self.evac_i += 1
        if eng == "act":
            kb.op("act", lambda e: e.activation(out=out, in_=in_, func=AF.Copy), reads=reads, writes=writes)
        else:
            kb.op(eng, lambda e: e.tensor_copy(out=out, in_=in_), reads=reads, writes=writes)

    def dump_x(self, name):
        if name in self.dbg_out:
            for i, tt in enumerate(self.tiles):
                self.kb.dma("sp", self.dbg_out[name][i * 128:(i + 1) * 128, :], self.xres(tt), self.d_io,
                            reads=[self.B_xres[tt]])

    def _consts(self):
        kb = self.kb
        Bc = self.B_const
        ident, ones_bf, ustrict = self.ident, self.ones_bf, self.ustrict
        kb.op("pool", lambda e: e.memset(ident[:], 0.0), writes=[Bc])
        kb.op("pool", lambda e: e.affine_select(out=ident[:], in_=ident[:], pattern=[[-1, 128]],
                                                compare_op=ALU.not_equal, fill=1.0, base=0,
                                                channel_multiplier=1), reads=[Bc], writes=[Bc])
        kb.op("pool", lambda e: e.memset(ones_bf[:], 1.0), writes=[Bc])
        kb.op("pool", lambda e: e.memset(ustrict[:], 1.0), writes=[Bc])
        kb.op("pool", lambda e: e.affine_select(out=ustrict[:], in_=ustrict[:], pattern=[[1, 128]],
                                                compare_op=ALU.is_gt, fill=0.0, base=0,
                                                channel_multiplier=-1), reads=[Bc], writes=[Bc])
        kb.op("pool", lambda e: e.iota(self.iota256[:], pattern=[[1, 256]], base=0, channel_multiplier=0,
                                       allow_small_or_imprecise_dtypes=True), writes=[Bc])
        kb.op("pool", lambda e: e.iota(self.pcol[:], pattern=[[128, 2]], base=0, channel_multiplier=1,
                                       allow_small_or_imprecise_dtypes=True), writes=[Bc])
        kb.op("pool", lambda e: e.iota(self.kidx[:], pattern=[[0, 128]], base=0, channel_multiplier=1,
                                       allow_small_or_imprecise_dtypes=True), writes=[Bc])
        kb.op("pool", lambda e: e.memset(self.epsc[:], LN_EPS), writes=[Bc])
        kb.dma("sp", self.brB[:], self.i_br.rearrange("l n -> (l n)").partition_broadcast(128), self.d_misc, writes=[Bc])
        kb.dma("sp", self.hv[:], self.i_hv, self.d_misc, writes=[Bc])

    def transpose_tile(self, src_ap, src_buf, dst3, dst_buf, col0, nchunks=16):
        kb, pp = self.kb, self.pp
        for c4 in range(nchunks // 4):
            j, pt, pb = pp.get()
            for k in range(4):
                c = c4 * 4 + k
                kb.op("pe", lambda e: e.transpose(out=pt[:, k * 128:(k + 1) * 128],
                                                  in_=src_ap[:, c * 128:(c + 1) * 128],
                                                  identity=self.ident[:]),
                      reads=[src_buf, self.B_const], writes=[pb])
            self.evac(dst3[:, c4 * 4:(c4 + 1) * 4, col0:col0 + 128],
                      pt[:].rearrange("p (k n) -> p k n", k=4), [pb], [dst_buf])

    def _layernorm(self, idx, make_xT=True):
        kb = self.kb
        kb.barrier()
        gB = view(self.CAT, 0, 2048)
        bB = view(self.CAT, 2048, 2048)
        Bg = Buf("lnparams")
        kb.dma("sp", gB, self.i_lng[self.L, idx].partition_broadcast(128), self.d_misc, writes=[Bg])
        kb.dma("sp", bB, self.i_lnb[self.L, idx].partition_broadcast(128), self.d_misc, writes=[Bg])
        self.B_xT = Buf("xT")
        xT = self.xT()
        st, mv, sc = self.stats, self.mv, self.sc
        Bs = self.B_small
        for tt in self.tiles:
            x = self.xres(tt)
            Bx = self.B_xres[tt]
            for i in range(4):
                kb.op("dve", lambda e: e.bn_stats(out=st[:, i, :], in_=x[:, i * 512:(i + 1) * 512]),
                      reads=[Bx], writes=[Bs])
            kb.op("dve", lambda e: e.bn_aggr(out=mv[:], in_=st[:].rearrange("p a b -> p (a b)")),
                  reads=[Bs], writes=[Bs])
            kb.op("act", lambda e: e.activation(out=sc[:, 0:1], in_=mv[:, 1:2], func=AF.Sqrt,
                                                bias=self.epsc[:], scale=1.0),
                  reads=[Bs, self.B_const], writes=[Bs])
            kb.op("dve", lambda e: e.reciprocal(out=sc[:, 0:1], in_=sc[:, 0:1]), reads=[Bs], writes=[Bs])
            kb.op("dve", lambda e: e.tensor_scalar(out=x, in0=x, scalar1=mv[:, 0:1], scalar2=sc[:, 0:1],
                                                   op0=ALU.subtract, op1=ALU.mult),
                  reads=[Bx, Bs], writes=[Bx])
            kb.op("pool", lambda e: e.tensor_tensor(out=x, in0=x, in1=gB, op=ALU.mult),
                  reads=[Bx, Bg], writes=[Bx])
            kb.op("dve", lambda e: e.tensor_tensor(out=x, in0=x, in1=bB, op=ALU.add),
                  reads=[Bx, Bg], writes=[Bx])
            if make_xT:
                self.transpose_tile(x, Bx, xT, self.B_xT, self.col0[tt])
        self.dump_x("ln%d_%d" % (self.L, idx))
        kb.barrier()

    def _wout_stage(self, w_ap):
        kb, pp = self.kb, self.pp
        catT = self.catT()
        for cg in range(4):
            W, Bw = self.wtile(self.wcols(w_ap, cg * 512))
            for tt in self.tiles:
                c0 = self.col0[tt]
                j, pt, pb = pp.get()
                for fc in range(16):
                    kb.op("pe", lambda e: e.matmul(pt[:], lhsT=catT[:, fc, c0:c0 + 128],
                                                   rhs=W[:, fc, :], start=(fc == 0), stop=(fc == 15)), sig=(fc == 15),
                          reads=[self.B_cat, Bw], writes=[pb])
                xs = self.xres(tt)[:, cg * 512:(cg + 1) * 512]
                kb.op("dve", lambda e: e.scalar_tensor_tensor(out=xs, in0=xs, scalar=ALPHA, in1=pt[:],
                                                              op0=ALU.mult, op1=ALU.add),
                      reads=[pb, self.B_xres[tt]], writes=[self.B_xres[tt]])

    def _store_out(self):
        kb = self.kb
        for i, tt in enumerate(self.tiles):
            kb.dma("sp", self.o_y[i * 128:(i + 1) * 128, :], self.xres(tt), self.d_io,
                   reads=[self.B_xres[tt]])

    def _mixer_even(self):
        kb, pp, nc = self.kb, self.pp, self.nc
        XR, FX = self.XRES, self.FLEX
        Bc = self.B_const
        xst = [view(XR, i * 2048, 2048) for i in range(2)]
        B_xst = [Buf("xst%d" % i) for i in range(2)]
        d_xst = [kb.dsem("xst%d" % i) for i in range(2)]
        xTc = [view(XR, 4096 + i * 4096, 4096, BF16, "p (c n) -> p c n", c=16) for i in range(2)]
        B_xTc = [Buf("xTc%d" % i) for i in range(2)]
        kTst = view(XR, 12288, 2048, BF16, "p (h n) -> p h n", h=8)
        B_kTst = Buf("kTst")
        d_kT = kb.dsem("kTscr")
        vst = view(XR, 14336, 2048, BF16, "p (t h n) -> p t h n", t=4, h=8)
        B_vst = Buf("vst")
        d_v = kb.dsem("vscr")
        B_kscr = Buf("kscr")
        B_vscr = Buf("vscr")
        lnvg = view(XR, 16384, 1024)
        lnvb = view(XR, 17408, 1024)
        qT = view(FX, 0, 4608, BF16, "p (h n) -> p h n", h=8)
        B_qT = Buf("qT")
        gvt = [view(FX, 4608 + i * 1024, 1024) for i in range(2)]
        B_gvt = [Buf("gvt%d" % i) for i in range(2)]
        vln = [view(FX, 6656 + i * 512, 512, BF16) for i in range(2)]
        B_vln = [Buf("vln%d" % i) for i in range(2)]
        wsT = view(FX, 7680, 512, BF16, "p (g n) -> p g n", g=8)
        bsB = view(FX, 8192, 1024)
        wsT_f = view(FX, 9216, 1024, F32, "p (g n) -> p g n", g=8)
        ztmp = view(FX, 9216, 512)
        lamt = view(FX, 10240, 256)
        B_ztmp = Buf("ztmp")
        B_par = Buf("evenpar")
        catT = self.catT()
        self.B_cat = Buf("cat")
        B_cat = self.B_cat
        st, mv, sc = self.stats, self.mv, self.sc
        Bs = self.B_small

        kb.dma("sp", wsT_f, self.i_wsT.rearrange("p (g n) -> p g n", g=8), self.d_misc, writes=[B_par])
        kb.dma("sp", bsB, self.i_bs.partition_broadcast(128), self.d_misc, writes=[B_par])
        kb.dma("sp", lnvg, self.i_lnvg.partition_broadcast(128), self.d_misc, writes=[B_par])
        kb.dma("sp", lnvb, self.i_lnvb.partition_broadcast(128), self.d_misc, writes=[B_par])
        kb.dma("sp", lamt, self.i_lam.partition_broadcast(128), self.d_misc, writes=[B_par])
        kb.dma("sp", self.subg[:], self.i_subg, self.d_misc, writes=[B_par])
        kb.op("dve", lambda e: e.memset(wsT_f[64:128, :, 0:64], 0.0), reads=[B_par], writes=[B_par])
        kb.op("dve", lambda e: e.tensor_copy(out=wsT, in_=wsT_f), reads=[B_par], writes=[B_par])
        lambda_init = 0.8 - 0.6 * math.exp(-0.3 * 0)
        lt, lc = lamt, self.lamc
        for i in range(2):
            kb.op("dve", lambda e: e.tensor_tensor(out=lt[:, i * 128:i * 128 + 64], in0=lt[:, i * 128:i * 128 + 64],
                                                   in1=lt[:, i * 128 + 64:i * 128 + 128], op=ALU.mult),
                  reads=[B_par], writes=[B_par])
            kb.op("dve", lambda e: e.reduce_sum(out=lc[:, i:i + 1], in_=lt[:, i * 128:i * 128 + 64],
                                                axis=mybir.AxisListType.X), reads=[B_par], writes=[B_par])
            kb.op("act", lambda e: e.activation(out=lc[:, 2 + i:3 + i], in_=lc[:, i:i + 1], func=AF.Exp),
                  reads=[B_par], writes=[B_par])
        kb.op("dve", lambda e: e.tensor_tensor(out=lc[:, 4:5], in0=lc[:, 3:4], in1=lc[:, 2:3], op=ALU.subtract),
              reads=[B_par], writes=[B_par])
        kb.op("dve", lambda e: e.tensor_scalar(out=lc[:, 4:5], in0=lc[:, 4:5], scalar1=-lambda_init, scalar2=None,
                                               op0=ALU.add), reads=[B_par], writes=[B_par])
        kb.op("dve", lambda e: e.tensor_scalar(out=self.subg[:], in0=self.subg[:], scalar1=1.0 - lambda_init,
                                               scalar2=None, op0=ALU.mult), reads=[B_par], writes=[B_par])

        win = self.i_win0
        x_i = 0
        for ci in range(8):
            own = ci >= 5
            xc, Bxc = xTc[ci % 2], B_xTc[ci % 2]
            for ti in range(4):
                s = x_i % 2
                x_i += 1
                row0 = ci * 512 + ti * 128
                kb.dma("sp", xst[s], self.i_xrel[row0:row0 + 128, :], d_xst[s], writes=[B_xst[s]])
                self.transpose_tile(xst[s], B_xst[s], xc, Bxc, ti * 128)
            for j in range(2):
                W, Bw = self.wtile(self.wcols(win, 3072 + j * 512))
                for hc in range(4):
                    h = 4 * j + hc
                    jj, pt, pb = pp.get()
                    for dc in range(16):
                        kb.op("pe", lambda e: e.matmul(pt[:], lhsT=W[:, dc, hc * 128:(hc + 1) * 128],
                                                       rhs=xc[:, dc, :], start=(dc == 0), stop=(dc == 15)), sig=(dc == 15),
                              reads=[Bw, Bxc], writes=[pb])
                    self.evac(kTst[:, h, :], pt[:], [pb], [B_kTst])
            kb.dma("sp", self.kT_scr[:, :, ci * 512:(ci + 1) * 512].rearrange("h p n -> p h n"), kTst, d_kT,
                   reads=[B_kTst], writes=[B_kscr])
            for j in range(2):
                W, Bw = self.wtile(self.wcols(win, 4096 + j * 512))
                for ti in range(4):
                    jj, pt, pb = pp.get()
                    for dc in range(16):
                        kb.op("pe", lambda e: e.matmul(pt[:], lhsT=xc[:, dc, ti * 128:(ti + 1) * 128],
                                                       rhs=W[:, dc, :], start=(dc == 0), stop=(dc == 15)), sig=(dc == 15),
                              reads=[Bw, Bxc], writes=[pb])
                    self.evac(vst[:, ti, 4 * j:4 * j + 4, :], pt[:].rearrange("p (h n) -> p h n", h=4),
                              [pb], [B_vst])
            for ti in range(4):
                kb.dma("sp", self.v_scr[:, :, 4 * ci + ti, :].rearrange("h p n -> p h n"), vst[:, ti, :, :], d_v,
                       reads=[B_vst], writes=[B_vscr])
            if not own:
                continue
            tis = [3] if ci == 5 else [0, 1, 2, 3]
            cl = 384 if ci == 5 else 0
            ncol = 128 * len(tis)
            oc0 = 0 if ci == 5 else 128 + (ci - 6) * 512
            for j in range(2):
                W, Bw = self.wtile(self.wcols(win, j * 512))
                for hc in range(4):
                    g = 4 * j + hc
                    jj, pt, pb = pp.get()
                    for dc in range(16):
                        kb.op("pe", lambda e: e.matmul(pt[:, 0:ncol], lhsT=W[:, dc, hc * 128:(hc + 1) * 128],
                                                       rhs=xc[:, dc, cl:cl + ncol], start=(dc == 0), stop=(dc == 15)), sig=(dc == 15),
                              reads=[Bw, Bxc], writes=[pb])
                    kb.op("act", lambda e: e.activation(out=catT[:, g, oc0:oc0 + ncol], in_=pt[:, 0:ncol],
                                                        func=AF.Gelu), reads=[pb], writes=[B_cat])
            Wg = []
            for j in range(2):
                Wg.append(self.wtile(self.wcols(win, 1024 + j * 512)))
            for tn, ti in enumerate(tis):
                s = ti % 2
                for j in range(2):
                    W, Bw = Wg[j]
                    jj, pt, pb = pp.get()
                    for dc in range(16):
                        kb.op("pe", lambda e: e.matmul(pt[:], lhsT=xc[:, dc, ti * 128:(ti + 1) * 128],
                                                       rhs=W[:, dc, :], start=(dc == 0), stop=(dc == 15)), sig=(dc == 15),
                              reads=[Bw, Bxc], writes=[pb])
                    kb.op("act", lambda e: e.activation(out=gvt[s][:, j * 512:(j + 1) * 512], in_=pt[:],
                                                        func=AF.Gelu), reads=[pb], writes=[B_gvt[s]])
                gv = gvt[s]
                for i in range(2):
                    kb.op("dve", lambda e: e.bn_stats(out=st[:, i, :], in_=gv[:, i * 512:(i + 1) * 512]),
                          reads=[B_gvt[s]], writes=[Bs])
                kb.op("dve", lambda e: e.bn_aggr(out=mv[:], in_=st[:, 0:2, :].rearrange("p a b -> p (a b)")),
                      reads=[Bs], writes=[Bs])
                kb.op("act", lambda e: e.activation(out=sc[:, 0:1], in_=mv[:, 1:2], func=AF.Sqrt,
                                                    bias=self.epsc[:], scale=1.0), reads=[Bs, Bc], writes=[Bs])
                kb.op("dve", lambda e: e.reciprocal(out=sc[:, 0:1], in_=sc[:, 0:1]), reads=[Bs], writes=[Bs])
                kb.op("dve", lambda e: e.tensor_scalar(out=gv, in0=gv, scalar1=mv[:, 0:1], scalar2=sc[:, 0:1],
                                                       op0=ALU.subtract, op1=ALU.mult),
                      reads=[B_gvt[s], Bs], writes=[B_gvt[s]])
                kb.op("dve", lambda e: e.tensor_tensor(out=gv, in0=gv, in1=lnvg, op=ALU.mult),
                      reads=[B_gvt[s], B_par], writes=[B_gvt[s]])
                kb.op("dve", lambda e: e.tensor_tensor(out=vln[s], in0=gv, in1=lnvb, op=ALU.add),
                      reads=[B_gvt[s], B_par], writes=[B_vln[s]])
                tok0 = oc0 + tn * 128
                for half in range(2):
                    jj, pt, pb = pp.get()
                    for gi in range(4):
                        g = half * 4 + gi
                        kb.op("pe", lambda e: e.matmul(pt[:, gi * 128:(gi + 1) * 128],
                                                       lhsT=vln[s][:, g * 128:(g + 1) * 128],
                                                       rhs=wsT[:, g, :], start=True, stop=True),
                              reads=[B_vln[s], B_par], writes=[pb])
                    kb.op("dve", lambda e: e.tensor_tensor(
                        out=ztmp.rearrange("p (g n) -> p g n", g=4),
                        in0=pt[:].rearrange("p (g n) -> p g n", g=4),
                        in1=bsB.rearrange("p (g n) -> p g n", g=8)[:, half * 4:half * 4 + 4, :], op=ALU.add),
                        reads=[pb, B_par], writes=[B_ztmp])
                    ao = catT[:, half * 4:half * 4 + 4, tok0:tok0 + 128]
                    kb.op("dve", lambda e: e.tensor_tensor(out=ao, in0=ao,
                                                           in1=ztmp.rearrange("p (g n) -> p g n", g=4),
                                                           op=ALU.mult),
                          reads=[B_ztmp, B_cat], writes=[B_cat])
            for j in range(2):
                W, Bw = self.wtile(self.wcols(win, 2048 + j * 512))
                for hc in range(4):
                    h = 4 * j + hc
                    jj, pt, pb = pp.get()
                    for dc in range(16):
                        kb.op("pe", lambda e: e.matmul(pt[:, 0:ncol], lhsT=W[:, dc, hc * 128:(hc + 1) * 128],
                                                       rhs=xc[:, dc, cl:cl + ncol], start=(dc == 0), stop=(dc == 15)), sig=(dc == 15),
                              reads=[Bw, Bxc], writes=[pb])
                    self.evac(qT[:, h, oc0:oc0 + ncol], pt[:, 0:ncol], [pb], [B_qT])
        kb.barrier()
        self.mark("phaseA")

        kTh = [view(XR, i * 2048, 2048, BF16) for i in range(2)]
        B_kTh = [Buf("kTh%d" % i) for i in range(2)]
        d_kTh = [kb.dsem("kTh%d" % i) for i in range(2)]
        vh = [view(XR, 4096 + i * 2048, 2048, BF16, "p (k n) -> p k n", k=32) for i in range(2)]
        B_vh = [Buf("vh%d" % i) for i in range(2)]
        d_vh = [kb.dsem("vh%d" % i) for i in range(2)]
        tabs = view(XR, 8192, 2560, F32, "p (a n) -> p a n", a=5)
        NSB, NPT = 3, 4
        sbt = [view(XR, 10752 + i * 512, 512) for i in range(2)] + [view(XR, 14592, 512)]
        B_sbt = [Buf("sbt%d" % i) for i in range(NSB)]
        pT = [view(XR, 11776 + i * 256, 256, BF16) for i in range(3)] + [view(XR, 17152, 256, BF16)]
        B_pT = [Buf("pT%d" % i) for i in range(NPT)]
        o0 = view(XR, 12544, 512)
        o1 = view(XR, 13056, 512)
        oo = view(XR, 13568, 512)
        rL = view(XR, 14080, 512)
        sq = view(XR, 15104, 256, BF16)
        rstd = view(XR, 15360, 512)
        abias = view(XR, 16384, 768)
        Lacc = [view(XR, 17408 + i * 512, 512) for i in range(2)] + [view(FX, 10496, 512)]
        B_Lacc = [Buf("Lacc%d" % i) for i in range(3)]
        B_o = Buf("att_o")
        B_tabs = Buf("att_tabs")
        kb.dma("sp", tabs, self.i_att.rearrange("a p n -> p a n"), self.d_misc, writes=[B_tabs])
        kb.dma("sp", abias, self.i_abias, self.d_misc, writes=[B_tabs])
        scale = 64 ** -0.5
        slopes = [2.0 ** (-8.0 * (i + 1) / 8) for i in range(8)]
        qranges = [(0, 128, 24, 23), (128, 512, 28, 24), (640, 512, 32, 28)]
        DEPTH = 2
        s_i = 0
        p_i = 0
        for h in range(8):
            hb = h % 2
            kb.dma("sp", kTh[hb], self.kT_scr[h], d_kTh[hb], reads=[B_kscr], writes=[B_kTh[hb]])
            kb.dma("sp", vh[hb], self.v_scr[h], d_vh[hb], reads=[B_vscr], writes=[B_vh[hb]])
            for qi, (qc0, qn, nkb, ov0) in enumerate(qranges):
                accO = [pp.get(pin=True) for m in range(2)]
                tl = [(kbi, m) for kbi in range(nkb) for m in range(2)]
                pend = []

                def stage1(kbi, m):
                    nonlocal s_i, p_i
                    ov = kbi - ov0
                    base = tabs[:, 0, 0:qn] if ov < 0 else tabs[:, 1 + ov, 0:qn]
                    jj, pt, pb = pp.get()
                    kb.op("pe", lambda e: e.matmul(pt[:, 0:qn],
                                                   lhsT=kTh[hb][m * 64:(m + 1) * 64, kbi * 128:(kbi + 1) * 128],
                                                   rhs=qT[m * 64:(m + 1) * 64, h, qc0:qc0 + qn],
                                                   start=True, stop=True),
                          reads=[B_kTh[hb], B_qT], writes=[pb])
                    si = s_i % NSB
                    s_i += 1
                    kb.op("dve", lambda e: e.scalar_tensor_tensor(out=sbt[si][:, 0:qn], in0=base,
                                                                  scalar=slopes[h] / scale,
                                                                  in1=pt[:, 0:qn], op0=ALU.mult, op1=ALU.add),
                          reads=[pb, B_tabs], writes=[B_sbt[si]])
                    pi = p_i % NPT
                    p_i += 1
                    bidx = h * 96 + qi * 32 + kbi
                    kb.op("act", lambda e: e.activation(out=pT[pi][:, 0:qn], in_=sbt[si][:, 0:qn], func=AF.Exp,
                                                        bias=abias[:, bidx:bidx + 1], scale=scale),
                          reads=[B_sbt[si], B_tabs], writes=[B_pT[pi]])
                    if m == 1:
                        leng, la, first = "pool", 1, (kbi == 0)
                    elif kbi % 2 == 0:
                        leng, la, first = "dve", 0, (kbi == 0)
                    else:
                        leng, la, first = "pool", 2, (kbi == 1)
                    if first:
                        kb.op(leng, lambda e: e.tensor_copy(out=Lacc[la][:, 0:qn], in_=pT[pi][:, 0:qn]),
                              reads=[B_pT[pi]], writes=[B_Lacc[la]])
                    else:
                        kb.op(leng, lambda e: e.tensor_tensor(out=Lacc[la][:, 0:qn], in0=Lacc[la][:, 0:qn],
                                                              in1=pT[pi][:, 0:qn], op=ALU.add),
                              reads=[B_pT[pi], B_Lacc[la]], writes=[B_Lacc[la]])
                    return pi

                def stage2(kbi, m, pi):
                    jo, po, pbo = accO[m]
                    kb.op("pe", lambda e: e.matmul(po[:, 0:qn], lhsT=vh[hb][:, kbi, :], rhs=pT[pi][:, 0:qn],
                                                   start=(kbi == 0), stop=(kbi == nkb - 1)),
                          reads=[B_vh[hb], B_pT[pi]], writes=[pbo])

                for (kbi, m) in tl:
                    pi = stage1(kbi, m)
                    pend.append((kbi, m, pi))
                    if len(pend) > DEPTH:
                        stage2(*pend.pop(0))
                while pend:
                    stage2(*pend.pop(0))
                outs = [o0, o1]
                kb.op("dve", lambda e: e.tensor_tensor(out=Lacc[0][:, 0:qn], in0=Lacc[0][:, 0:qn], in1=Lacc[2][:, 0:qn],
                                                       op=ALU.add), reads=[B_Lacc[0], B_Lacc[2]], writes=[B_Lacc[0]])
                for m in range(2):
                    jo, po, pbo = accO[m]
                    kb.op("act", lambda e: e.activation(out=sq[:, 0:qn], in_=Lacc[m][:, 0:qn], func=AF.Copy),
                          reads=[B_Lacc[m], B_o], writes=[B_o])
                    jj, pl, pbl = pp.get()
                    kb.op("pe", lambda e: e.matmul(pl[:, 0:qn], lhsT=self.ones_bf[:], rhs=sq[:, 0:qn],
                                                   start=True, stop=True), reads=[Bc, B_o], writes=[pbl])
                    kb.op("dve", lambda e: e.tensor_scalar(out=rL[:, 0:qn], in0=pl[:, 0:qn], scalar1=1e-30,
                                                           scalar2=None, op0=ALU.max), reads=[pbl, B_o], writes=[B_o])
                    kb.op("dve", lambda e: e.reciprocal(out=rL[:, 0:qn], in_=rL[:, 0:qn]),
                          reads=[B_o], writes=[B_o])
                    kb.op("dve", lambda e: e.tensor_tensor(out=outs[m][:, 0:qn], in0=po[:, 0:qn], in1=rL[:, 0:qn],
                                                           op=ALU.mult), reads=[pbo, B_o], writes=[B_o])
                kb.op("dve", lambda e: e.scalar_tensor_tensor(out=oo[:, 0:qn], in0=o1[:, 0:qn],
                                                              scalar=self.lamc[:, 4:5], in1=o0[:, 0:qn],
                                                              op0=ALU.mult, op1=ALU.add),
                      reads=[B_o, B_par], writes=[B_o])
                kb.op("dve", lambda e: e.tensor_tensor(out=sq[:, 0:qn], in0=oo[:, 0:qn], in1=oo[:, 0:qn], op=ALU.mult),
                      reads=[B_o], writes=[B_o])
                for a in accO:
                    pp.unpin(a[0])
                jj, pt, pb = pp.get()
                kb.op("pe", lambda e: e.matmul(pt[:, 0:qn], lhsT=self.ones_bf[:], rhs=sq[:, 0:qn], start=True, stop=True),
                      reads=[Bc, B_o], writes=[pb])
                kb.op("act", lambda e: e.activation(out=rstd[:, 0:qn], in_=pt[:, 0:qn], func=AF.Sqrt, bias=self.epsc[:],
                                                    scale=1.0 / 128), reads=[pb, Bc], writes=[B_o])
                kb.op("dve", lambda e: e.reciprocal(out=rstd[:, 0:qn], in_=rstd[:, 0:qn]), reads=[B_o], writes=[B_o])
                kb.op("dve", lambda e: e.scalar_tensor_tensor(out=catT[:, 8 + h, qc0:qc0 + qn], in0=oo[:, 0:qn],
                                                              scalar=self.subg[:, 0:1], in1=rstd[:, 0:qn],
                                                              op0=ALU.mult, op1=ALU.mult),
                      reads=[B_o, B_par], writes=[B_cat])
        kb.barrier()
        for tt in self.tiles:
            kb.dma("sp", self.xres(tt), self.i_xrel[2944 + tt * 128:2944 + (tt + 1) * 128, :], self.d_io,
                   writes=[self.B_xres[tt]])
        for tt in self.tiles:
            self.B_xres[tt].w = (self.d_io.key, self.d_io.cnt)

    def _mixer_odd(self):
        kb, pp = self.kb, self.pp
        FX = self.FLEX
        self.B_cat = Buf("cat")
        B_cat = self.B_cat
        xT = self.xT()
        ptab = view(FX, 9216, 1024, BF16, "p (a j n) -> p a j n", a=4, j=4)
        invc = view(FX, 10240, 512, F32, "p (j n) -> p j n", j=4)
        scaleT = view(FX, 10752, 16)
        B_par = Buf("oddpar")
        kb.dma("pool", ptab.rearrange("p a j n -> p (a j n)"), self.i_ptab, self.d_misc, writes=[B_par])
        kb.dma("sp", invc, self.i_pinv.rearrange("p (j n) -> p j n", j=4), self.d_misc, writes=[B_par])
        kb.dma("sp", scaleT, self.i_scale, self.d_misc, writes=[B_par])
        h_bf = view(self.CAT, 0, 9216, BF16, "p (t n) -> p t n", t=NT9)
        B_h = Buf("h_bf")
        for cg in range(4):
            W, Bw = self.wtile(self.wcols(self.i_win1, cg * 512))
            for tt in range(NT9):
                jj, pt, pb = pp.get()
                for dc in range(16):
                    kb.op("pe", lambda e: e.matmul(pt[:], lhsT=xT[:, dc, tt * 128:(tt + 1) * 128], rhs=W[:, dc, :],
                                                   start=(dc == 0), stop=(dc == 15)), sig=(dc == 15),
                          reads=[Bw, self.B_xT], writes=[pb])
                self.evac(h_bf[:, tt, cg * 512:(cg + 1) * 512], pt[:], [pb], [B_h])
        kb.barrier()
        pooledT = view(FX, 0, 8192, BF16, "p (c n) -> p c n", c=16)
        B_pool = Buf("pooledT")
        wins = (2, 4, 8, 16)
        for tt in range(1, NT9):
            first = (tt == 1)
            c0 = (tt - 1) * 128
            for j in range(4):
                jj, pt, pb = pp.get()
                for k in range(4):
                    cc = 4 * j + k
                    acur = ptab[:, 0, j, :] if first else ptab[:, 1, j, :]
                    aprev = ptab[:, 2, j, :] if first else ptab[:, 3, j, :]
                    kb.op("pe", lambda e: e.matmul(pt[:, k * 128:(k + 1) * 128],
                                                   lhsT=h_bf[:, tt - 1, cc * 128:(cc + 1) * 128], rhs=aprev,
                                                   start=True, stop=False),
                          reads=[B_h, B_par], writes=[pb])
                    kb.op("pe", lambda e: e.matmul(pt[:, k * 128:(k + 1) * 128],
                                                   lhsT=h_bf[:, tt, cc * 128:(cc + 1) * 128], rhs=acur,
                                                   start=False, stop=True),
                          reads=[B_h, B_par], writes=[pb])
                dst = pooledT[:, 4 * j:4 * j + 4, c0:c0 + 128]
                src = pt[:].rearrange("p (k n) -> p k n", k=4)
                if first:
                    for k in range(4):
                        kb.op("dve", lambda e: e.tensor_tensor(out=dst[:, k, :], in0=src[:, k, :], in1=invc[:, j, :],
                                                               op=ALU.mult), reads=[pb, B_par], writes=[B_pool])
                else:
                    kb.op("act", lambda e: e.activation(out=dst, in_=src, func=AF.Copy, scale=1.0 / wins[j]),
                          reads=[pb], writes=[B_pool])
        kb.barrier()
        catT = view(self.CAT, 0, 8192, BF16, "p (c n) -> p c n", c=16)
        Wg, Bwg = self.wtile(self.i_wgrp.rearrange("(c p) n -> p c n", p=128))
        for j in range(4):
            for occ in range(4):
                for ch in range(2):
                    jj, pt, pb = pp.get()
                    for cc in range(4):
                        kb.op("pe", lambda e: e.matmul(pt[:], lhsT=Wg[:, 4 * j + cc, occ * 128:(occ + 1) * 128],
                                                       rhs=pooledT[:, 4 * j + cc, ch * 512:(ch + 1) * 512],
                                                       start=(cc == 0), stop=(cc == 3)),
                              reads=[Bwg, B_pool], writes=[pb])
                    oc = 4 * j + occ
                    kb.op("dve", lambda e: e.tensor_scalar(out=catT[:, oc, ch * 512:(ch + 1) * 512], in0=pt[:],
                                                           scalar1=scaleT[:, oc:oc + 1], scalar2=None, op0=ALU.mult),
                          reads=[pb, B_par], writes=[B_cat])
        kb.barrier()

    def _xattn(self):
        kb, pp = self.kb, self.pp
        CT = self.CAT
        Bc = self.B_const
        L = self.L
        nt = self.ntok
        xT = self.xT()
        memT = view(CT, 0, 2048, BF16, "p (c n) -> p c n", c=16)
        qTx = view(CT, 2048, 2 * nt, BF16, "p (h n) -> p h n", h=4)
        o1_ = 2048 + 2 * nt
        oTx = view(CT, o1_, 2 * nt, BF16, "p (h n) -> p h n", h=4)
        o2_ = o1_ + 2 * nt
        kTm = view(CT, o2_, 512, BF16, "p (h n) -> p h n", h=4)
        vm = view(CT, o2_ + 512, 512, BF16, "p (m n) -> p m n", m=2)
        pT = [view(CT, o2_ + 1024 + i * 256, 256, BF16) for i in range(2)]
        rL = view(CT, o2_ + 1536, 512)
        assert o2_ + 2048 <= 9216
        B_memT, B_q, B_k, B_v, B_o, B_rL = [Buf(n) for n in ("memT", "qTx", "kTm", "vm", "oTx", "rLx")]
        B_pT = [Buf("pTx%d" % i) for i in range(2)]
        for mb in range(2):
            mst, B_mst, d_mst = self.ring_f32(2048)
            kb.dma("sp", mst, self.i_mem[mb * 128:(mb + 1) * 128, :], d_mst, writes=[B_mst])
            self.transpose_tile(mst, B_mst, memT, B_memT, mb * 128)
        Wq, Bwq = self.wtile(self.wcols(self.i_wq[L], 0))
        for h in range(4):
            for (c0, n) in self.ranges:
                jj, pt, pb = pp.get()
                for dc in range(16):
                    kb.op("pe", lambda e: e.matmul(pt[:, 0:n], lhsT=Wq[:, dc, h * 128:(h + 1) * 128],
                                                   rhs=xT[:, dc, c0:c0 + n],
                                                   start=(dc == 0), stop=(dc == 15)), sig=(dc == 15),
                          reads=[Bwq, self.B_xT], writes=[pb])
                self.evac(qTx[:, h, c0:c0 + n], pt[:, 0:n], [pb], [B_q])
        Wk, Bwk = self.wtile(self.wcols(self.i_wk[L], 0))
        for h in range(4):
            jj, pt, pb = pp.get()
            for dc in range(16):
                kb.op("pe", lambda e: e.matmul(pt[:, 0:256], lhsT=Wk[:, dc, h * 128:(h + 1) * 128], rhs=memT[:, dc, :],
                                               start=(dc == 0), stop=(dc == 15)), sig=(dc == 15),
                      reads=[Bwk, B_memT], writes=[pb])
            self.evac(kTm[:, h, :], pt[:, 0:256], [pb], [B_k])
        Wv, Bwv = self.wtile(self.wcols(self.i_wv[L], 0))
        for mb in range(2):
            jj, pt, pb = pp.get()
            for dc in range(16):
                kb.op("pe", lambda e: e.matmul(pt[:], lhsT=memT[:, dc, mb * 128:(mb + 1) * 128], rhs=Wv[:, dc, :],
                                               start=(dc == 0), stop=(dc == 15)), sig=(dc == 15),
                      reads=[Bwv, B_memT], writes=[pb])
            self.evac(vm[:, mb, :], pt[:], [pb], [B_v])
        scale = 128 ** -0.5
        p_i = 0
        for h in range(4):
            for (c0, n) in self.ranges:
                jo, po, pbo = pp.get(pin=True)
                jl, pl, pbl = pp.get(pin=True)
                for mb in range(2):
                    jj, pt, pb = pp.get()
                    kb.op("pe", lambda e: e.matmul(pt[:, 0:n], lhsT=kTm[:, h, mb * 128:(mb + 1) * 128],
                                                   rhs=qTx[:, h, c0:c0 + n], start=True, stop=True),
                          reads=[B_k, B_q], writes=[pb])
                    pi = p_i % 2
                    p_i += 1
                    kb.op("act", lambda e: e.activation(out=pT[pi][:, 0:n], in_=pt[:, 0:n], func=AF.Exp, scale=scale),
                          reads=[pb], writes=[B_pT[pi]])
                    kb.op("pe", lambda e: e.matmul(po[:, 0:n], lhsT=vm[:, mb, h * 128:(h + 1) * 128], rhs=pT[pi][:, 0:n],
                                                   start=(mb == 0), stop=(mb == 1)),
                          reads=[B_v, B_pT[pi]], writes=[pbo])
                    kb.op("pe", lambda e: e.matmul(pl[:, 0:n], lhsT=self.ones_bf[:], rhs=pT[pi][:, 0:n],
                                                   start=(mb == 0), stop=(mb == 1)),
                          reads=[Bc, B_pT[pi]], writes=[pbl])
                kb.op("dve", lambda e: e.reciprocal(out=rL[:, 0:n], in_=pl[:, 0:n]), reads=[pbl], writes=[B_rL])
                kb.op("dve", lambda e: e.tensor_tensor(out=oTx[:, h, c0:c0 + n], in0=po[:, 0:n], in1=rL[:, 0:n],
                                                       op=ALU.mult), reads=[pbo, B_rL], writes=[B_o])
                pp.unpin(jo)
                pp.unpin(jl)
        Wo, Bwo = self.wtile(self.i_wo[L].rearrange("(c p) n -> p c n", p=128))
        for tt in self.tiles:
            c0 = self.col0[tt]
            for cg in range(4):
                jj, pt, pb = pp.get()
                for hc in range(4):
                    kb.op("pe", lambda e: e.matmul(pt[:], lhsT=oTx[:, hc, c0:c0 + 128],
                                                   rhs=Wo[:, hc, cg * 512:(cg + 1) * 512],
                                                   start=(hc == 0), stop=(hc == 3)),
                          reads=[Bwo, B_o], writes=[pb])
                xs = self.xres(tt)[:, cg * 512:(cg + 1) * 512]
                kb.op("dve", lambda e: e.scalar_tensor_tensor(out=xs, in0=xs, scalar=ALPHA, in1=pt[:],
                                                              op0=ALU.mult, op1=ALU.add),
                      reads=[pb, self.B_xres[tt]], writes=[self.B_xres[tt]])

    def _moe(self):
        kb, pp = self.kb, self.pp
        CT, FX = self.CAT, self.FLEX
        Bc = self.B_const
        L = self.L
        tiles = self.tiles
        ntl = len(tiles)
        nt = self.ntok
        xT = self.xT()
        x_bf = view(CT, 0, ntl * 1024, BF16, "p (t n) -> p t n", t=ntl)
        B_xbf = Buf("x_bf")
        lg, maskt, mask_bf, gate, rankm = self.lg, self.maskt, self.mask_bf, self.gate, self.rankm
        ex, top8, sc = self.ex, self.top8, self.sc
        Br = Buf("router")
        wr, B_wr = self.wtile(self.i_wr[L].rearrange("(c p) n -> p c n", p=128))
        brB = self.brB[:, L * NE:(L + 1) * NE]
        for i, tt in enumerate(tiles):
            c0 = self.col0[tt]
            self.evac(x_bf[:, i, :], self.xres(tt), [self.B_xres[tt]], [B_xbf], eng="act")
            jj, pt, pb = pp.get()
            for dc in range(16):
                kb.op("pe", lambda e: e.matmul(pt[:, 0:NE], lhsT=xT[:, dc, c0:c0 + 128], rhs=wr[:, dc, :],
                                               start=(dc == 0), stop=(dc == 15)), sig=(dc == 15),
                      reads=[self.B_xT, B_wr], writes=[pb])
            l = lg[:, i, :]
            kb.op("dve", lambda e: e.tensor_tensor(out=l, in0=pt[:, 0:NE], in1=brB, op=ALU.add),
                  reads=[pb, Bc], writes=[Br])
            kb.op("dve", lambda e: e.max(out=top8[:], in_=l), reads=[Br], writes=[Br])
            kb.op("dve", lambda e: e.tensor_scalar(out=maskt[:, i, :], in0=l, scalar1=top8[:, 3:4], scalar2=None,
                                                   op0=ALU.is_ge), reads=[Br], writes=[Br])
            if L == 0 and tt == 0:
                kb.op("dve", lambda e: e.tensor_scalar(out=maskt[:, i, :], in0=maskt[:, i, :], scalar1=self.hv[:, 0:1],
                                                       scalar2=None, op0=ALU.mult), reads=[Br, Bc], writes=[Br])
            kb.op("dve", lambda e: e.tensor_scalar(out=sc[:, 1:2], in0=top8[:, 0:1], scalar1=-1.0, scalar2=None,
                                                   op0=ALU.mult), reads=[Br], writes=[Br])
            kb.op("act", lambda e: e.activation(out=ex[:], in_=l, func=AF.Exp, bias=sc[:, 1:2], scale=1.0),
                  reads=[Br], writes=[Br])
            kb.op("dve", lambda e: e.tensor_tensor(out=ex[:], in0=ex[:], in1=maskt[:, i, :], op=ALU.mult),
                  reads=[Br], writes=[Br])
            kb.op("dve", lambda e: e.reduce_sum(out=sc[:, 2:3], in_=ex[:], axis=mybir.AxisListType.X),
                  reads=[Br], writes=[Br])
            kb.op("dve", lambda e: e.tensor_scalar(out=sc[:, 2:3], in0=sc[:, 2:3], scalar1=1e-30, scalar2=None,
                                                   op0=ALU.max), reads=[Br], writes=[Br])
            kb.op("dve", lambda e: e.reciprocal(out=sc[:, 2:3], in_=sc[:, 2:3]), reads=[Br], writes=[Br])
            kb.op("dve", lambda e: e.tensor_scalar(out=gate[:, i, :], in0=ex[:], scalar1=sc[:, 2:3], scalar2=None,
                                                   op0=ALU.mult), reads=[Br], writes=[Br])
            kb.op("dve", lambda e: e.tensor_copy(out=mask_bf[:, i, :], in_=maskt[:, i, :]), reads=[Br], writes=[Br])
        for i in range(ntl):
            jj, pt, pb = pp.get()
            for t2 in range(i):
                kb.op("pe", lambda e: e.matmul(pt[:, 0:NE], lhsT=self.ones_bf[:], rhs=mask_bf[:, t2, :],
                                               start=(t2 == 0), stop=False), reads=[Br, Bc], writes=[pb])
            kb.op("pe", lambda e: e.matmul(pt[:, 0:NE], lhsT=self.ustrict[:], rhs=mask_bf[:, i, :],
                                           start=(i == 0), stop=True), reads=[Br, Bc], writes=[pb])
            kb.op("dve", lambda e: e.scalar_tensor_tensor(out=rankm[:, i, :], in0=pt[:, 0:NE], scalar=1.0,
                                                          in1=maskt[:, i, :], op0=ALU.add, op1=ALU.mult),
                  reads=[pb, Br], writes=[Br])
            kb.op("dve", lambda e: e.tensor_scalar(out=rankm[:, i, :], in0=rankm[:, i, :], scalar1=-1.0,
                                                   scalar2=None, op0=ALU.add), reads=[Br], writes=[Br])
        for tt in tiles:
            kb.op("pool", lambda e: e.tensor_scalar(out=self.xres(tt), in0=self.xres(tt), scalar1=ALPHA,
                                                    scalar2=None, op0=ALU.mult),
                  reads=[self.B_xres[tt], B_xbf], writes=[self.B_xres[tt]])
        kb.barrier()
        xselT = view(FX, 0, 2048, BF16, "p (c n) -> p c n", c=16)
        actT = view(FX, 2048, 2048, BF16, "p (c n) -> p c n", c=16)
        ye = [view(FX, 4096 + i * 512, 512, BF16, "p (s n) -> p s n", s=2) for i in range(2)]
        S = view(FX, 5120, ntl * 128, BF16, "p (t n) -> p t n", t=ntl)
        ST = view(FX, 6272, nt, BF16, "p (s n) -> p s n", s=2)
        gl = view(FX, 7424, 1024, F32, "p (k n) -> p k n", k=4)
        rankmT = self.FLEX[0:32, 8448:8448 + nt]
        esel = self.FLEX[0:32, 9600:9728]
        gS = [view(FX, 9728 + i * 256, 256) for i in range(2)]
        sS = [view(FX, 10240 + i * 256, 256) for i in range(2)]
        lS = view(FX, 10752, 256)
        assert 10752 + 256 <= FLEXW
        B_xsel, B_act, B_S, B_ST, B_gl, B_rT, B_gT, B_esel, B_lS = [
            Buf(n) for n in ("xselT", "actT", "S", "ST", "gl", "rankmT", "gateT", "esel", "lS")]
        B_ye = [Buf("ye%d" % i) for i in range(2)]
        B_gS = [Buf("gS%d" % i) for i in range(2)]
        B_sS = [Buf("sS%d" % i) for i in range(2)]
        B_bg = [Buf("bg%d" % i) for i in range(2)]
        if not hasattr(self, "d_bg"):
            self.d_bg = [kb.dsem("bg%d" % i) for i in range(2)]
        d_bg = self.d_bg

        def transpose_table(src, dst, Bd):
            for b0 in range(0, ntl, 4):
                nb = min(4, ntl - b0)
                jj, pt, pb = pp.get()
                for k in range(nb):
                    kb.op("pe", lambda e: e.transpose(out=pt[0:32, k * 128:(k + 1) * 128], in_=src[:, b0 + k, :],
                                                      identity=self.ident[:]), reads=[Br, Bc], writes=[pb])
                self.evac(dst[:, b0 * 128:(b0 + nb) * 128], pt[0:32, 0:nb * 128], [pb], [Bd], eng="dve")

        transpose_table(rankm, rankmT, B_rT)
        g_i = 0
        y_i = 0

        def build_S(ei):
            for i in range(ntl):
                kb.op("dve", lambda e: e.tensor_scalar(out=S[:, i, :], in0=self.iota256[:],
                                                       scalar1=rankm[:, i, ei:ei + 1], scalar2=None,
                                                       op0=ALU.is_equal), reads=[Br, Bc], writes=[B_S])

        def gather_part(d2s):
            for d2 in d2s:
                jj, pt, pb = pp.get()
                for k in range(2):
                    dc = 2 * d2 + k
                    for i in range(ntl):
                        kb.op("pe", lambda e: e.matmul(pt[:, k * 256:(k + 1) * 256],
                                                       lhsT=x_bf[:, i, dc * 128:(dc + 1) * 128], rhs=S[:, i, :],
                                                       start=(i == 0), stop=(i == ntl - 1)), sig=(i == ntl - 1),
                              reads=[B_xbf, B_S], writes=[pb])
                self.evac(xselT[:, 2 * d2:2 * d2 + 2, :], pt[:].rearrange("p (k n) -> p k n", k=2), [pb], [B_xsel])

        build_S(0)
        gather_part(range(8))
        for ei in range(NE):
            eb = ei % 2
            bg = self.bgt[:, eb, :]
            kb.dma("sp", bg, self.i_bgu[L, :, ei * 32:(ei + 1) * 32], d_bg[eb], writes=[B_bg[eb]])
            def build_ST():
                kb.op("dve", lambda e: e.tensor_scalar(out=esel, in0=self.kidx[:], scalar1=float(ei), scalar2=None,
                                                       op0=ALU.is_equal), reads=[Bc], writes=[B_esel])
                for (c0, n) in self.ranges:
                    jj, pt, pb = pp.get()
                    kb.op("pe", lambda e: e.matmul(pt[:, 0:n], lhsT=esel, rhs=rankmT[:, c0:c0 + n],
                                                   start=True, stop=True), reads=[B_esel, B_rT], writes=[pb])
                    for sh in range(2):
                        kb.op("dve", lambda e: e.tensor_scalar(out=ST[:, sh, c0:c0 + n], in0=pt[:, 0:n],
                                                               scalar1=self.pcol[:, sh:sh + 1], scalar2=None,
                                                               op0=ALU.is_equal), reads=[pb, Bc], writes=[B_ST])
            for j in range(4):
                if j == 2:
                    build_ST()
                Wg_, Bwg = self.wtile(self.wcols(self.i_wgu[L, ei], j * 512))
                Wl_, Bwl = self.wtile(self.wcols(self.i_wgu[L, ei], 2048 + j * 512))
                for k2 in range(2):
                    jj, pt, pb = pp.get()
                    for k1 in range(2):
                        k = 2 * k2 + k1
                        for dc in range(16):
                            kb.op("pe", lambda e: e.matmul(pt[:, k1 * 256:(k1 + 1) * 256],
                                                           lhsT=Wg_[:, dc, k * 128:(k + 1) * 128], rhs=xselT[:, dc, :],
                                                           start=(dc == 0), stop=(dc == 15)), sig=(dc == 15),
                                  reads=[Bwg, B_xsel], writes=[pb])
                    for k1 in range(2):
                        k = 2 * k2 + k1
                        c = 4 * j + k
                        gi = g_i % 2
                        g_i += 1
                        kb.op("dve", lambda e: e.tensor_scalar(out=gS[gi], in0=pt[:, k1 * 256:(k1 + 1) * 256],
                                                               scalar1=bg[:, c:c + 1], scalar2=SW_LIMIT,
                                                               op0=ALU.add, op1=ALU.min),
                              reads=[pb, B_bg[eb]], writes=[B_gS[gi]])
                        kb.op("act", lambda e: e.activation(out=sS[gi], in_=gS[gi], func=AF.Sigmoid, scale=SW_ALPHA),
                              reads=[B_gS[gi]], writes=[B_sS[gi]])
                        kb.op("dve", lambda e: e.tensor_tensor(out=gl[:, k, :], in0=gS[gi], in1=sS[gi], op=ALU.mult),
                              reads=[B_gS[gi], B_sS[gi]], writes=[B_gl])
                for k2 in range(2):
                    jj, pt, pb = pp.get()
                    for k1 in range(2):
                        k = 2 * k2 + k1
                        for dc in range(16):
                            kb.op("pe", lambda e: e.matmul(pt[:, k1 * 256:(k1 + 1) * 256],
                                                           lhsT=Wl_[:, dc, k * 128:(k + 1) * 128], rhs=xselT[:, dc, :],
                                                           start=(dc == 0), stop=(dc == 15)), sig=(dc == 15),
                                  reads=[Bwl, B_xsel], writes=[pb])
                    for k1 in range(2):
                        k = 2 * k2 + k1
                        c = 4 * j + k
                        kb.op("dve", lambda e: e.tensor_scalar(out=lS, in0=pt[:, k1 * 256:(k1 + 1) * 256],
                                                               scalar1=bg[:, 16 + c:17 + c], scalar2=SW_LIMIT,
                                                               op0=ALU.add, op1=ALU.min),
                              reads=[pb, B_bg[eb]], writes=[B_lS])
                        kb.op("dve", lambda e: e.tensor_scalar(out=lS, in0=lS, scalar1=-SW_LIMIT, scalar2=1.0,
                                                               op0=ALU.max, op1=ALU.add),
                              reads=[B_lS], writes=[B_lS])
                        kb.op("dve", lambda e: e.tensor_tensor(out=actT[:, c, :], in0=gl[:, k, :], in1=lS,
                                                               op=ALU.mult),
                              reads=[B_gl, B_lS], writes=[B_act])
            nxt = ei + 1 < NE
            if nxt:
                build_S(ei + 1)
            for cg in range(4):
                Wd_, Bwd = self.wtile(self.wcols(self.i_wd[L, ei], cg * 512))
                yi = y_i % 2
                y_i += 1
                for sh in range(2):
                    jj, pt, pb = pp.get()
                    for fc in range(16):
                        kb.op("pe", lambda e: e.matmul(pt[:], lhsT=actT[:, fc, sh * 128:(sh + 1) * 128], rhs=Wd_[:, fc, :],
                                                       start=(fc == 0), stop=(fc == 15)), sig=(fc == 15),
                              reads=[Bwd, B_act], writes=[pb])
                    self.evac(ye[yi][:, sh, :], pt[:], [pb], [B_ye[yi]], eng="act")
                if nxt:
                    gather_part([2 * cg, 2 * cg + 1])
                for i, tt in enumerate(tiles):
                    c0 = self.col0[tt]
                    jj, pt, pb = pp.get()
                    for sh in range(2):
                        kb.op("pe", lambda e: e.matmul(pt[:], lhsT=ST[:, sh, c0:c0 + 128], rhs=ye[yi][:, sh, :],
                                                       start=(sh == 0), stop=(sh == 1)),
                              reads=[B_ST, B_ye[yi]], writes=[pb])
                    xs = self.xres(tt)[:, cg * 512:(cg + 1) * 512]
                    kb.op("dve", lambda e: e.scalar_tensor_tensor(out=xs, in0=pt[:], scalar=gate[:, i, ei:ei + 1],
                                                                  in1=xs, op0=ALU.mult, op1=ALU.add),
                          reads=[pb, Br, self.B_xres[tt]], writes=[self.B_xres[tt]])
        kb.barrier()
        gateT = self.FLEX[0:32, 0:nt]
        bd = self.FLEX[0:32, 1152:1152 + 2048]
        B_bd = Buf("bd")
        kb.dma("sp", bd, self.i_bd[L], self.d_misc, writes=[B_bd])
        transpose_table(gate, gateT, B_gT)
        for tt in tiles:
            c0 = self.col0[tt]
            for cg in range(4):
                jj, pt, pb = pp.get()
                kb.op("pe", lambda e: e.matmul(pt[:], lhsT=gateT[:, c0:c0 + 128],
                                               rhs=bd[:, cg * 512:(cg + 1) * 512], start=True, stop=True),
                      reads=[B_gT, B_bd], writes=[pb])
                xs = self.xres(tt)[:, cg * 512:(cg + 1) * 512]
                kb.op("dve", lambda e: e.tensor_tensor(out=xs, in0=xs, in1=pt[:], op=ALU.add),
                      reads=[pb, self.B_xres[tt]], writes=[self.B_xres[tt]])


_PROG = {}


def _get_prog(dbg=None):
    key = tuple(dbg or ())
    if key not in _PROG:
        p = Prog(dbg)
        p.build()
        _PROG[key] = p
    return _PROG[key]


def _att_tables():
    ki = np.arange(128, dtype=np.float64)[:, None]
    qi = np.arange(512, dtype=np.float64)[None, :]
    tabs = np.zeros((5, 128, 512), np.float32)
    tabs[0] = ki - qi
    for o in range(4):
        kpos = 128 * o + ki
        allowed = (64 * np.floor(kpos / 64)) <= qi
        tabs[1 + o] = np.where(allowed, -np.abs(qi - kpos), NEGBIG)
    return tabs


def _att_bias(r):
    slopes = [2.0 ** (-8.0 * (i + 1) / 8) for i in range(8)]
    qranges = [(2944, 24, 23), (3072, 28, 24), (3584, 32, 28)]
    b = np.zeros((8, 3, 32), np.float32)
    first_valid = (3072 - 1024 * r) // 128
    for h in range(8):
        for qi, (qs, nkb, ov0) in enumerate(qranges):
            for kb_ in range(32):
                if kb_ >= nkb:
                    v = NEGBIG
                elif kb_ >= ov0:
                    v = 0.0
                else:
                    v = -slopes[h] * (qs - 128 * kb_)
                if kb_ < first_valid:
                    v = NEGBIG
                b[h, qi, kb_] = v
    return np.ascontiguousarray(np.broadcast_to(b.reshape(1, 768), (128, 768)))


def _pool_tables(r):
    wins = (2, 4, 8, 16)
    s = np.arange(128)[:, None]
    t = np.arange(128)[None, :]
    tabs = np.zeros((128, 4, 4, 128), np.float32)
    invc = np.zeros((128, 4, 128), np.float32)
    for j, w in enumerate(wins):
        band = ((s <= t) & (s > t - w)).astype(np.float32)
        cnt_first = np.minimum(np.arange(128) + 1, w).astype(np.float32) if r == 0 else np.full(128, float(w), np.float32)
        prev = ((s - 128) > (t - w)).astype(np.float32)
        tabs[:, 0, j, :] = band - np.eye(128, dtype=np.float32) * cnt_first[None, :]
        tabs[:, 1, j, :] = band - np.eye(128, dtype=np.float32) * float(w)
        tabs[:, 2, j, :] = prev if r > 0 else 0.0
        tabs[:, 3, j, :] = prev
        invc[:, j, :] = (1.0 / cnt_first)[None, :]
    return tabs.reshape(128, -1), invc.reshape(128, -1)


def _make_inputs(inp):
    x = inp["x"]
    tabs = _att_tables()
    w_sT = np.ascontiguousarray(inp["even_w_s"][0].transpose(2, 0, 1).reshape(128, 8 * 128))
    lam = np.ascontiguousarray(np.concatenate([inp["even_lam_q1"][0], inp["even_lam_k1"][0],
                                               inp["even_lam_q2"][0], inp["even_lam_k2"][0]]))
    bgu = inp["moe_b_gu"]
    bguT = np.ascontiguousarray(bgu.reshape(2, NE, 32, 128).transpose(0, 3, 1, 2).reshape(2, 128, NE * 32))
    scaleT = np.ascontiguousarray(inp["odd_scale"][0].reshape(16, 128).T)
    shared = {
        "att_tabs": tabs,
        "even_w_in": inp["even_w_in"][0], "even_ln_v_g": inp["even_ln_v_g"][0], "even_ln_v_b": inp["even_ln_v_b"][0],
        "w_sT": w_sT, "even_b_s": np.ascontiguousarray(inp["even_b_s"][0].reshape(-1)),
        "lam_vecs": lam, "even_subln_g": np.ascontiguousarray(inp["even_subln_g"][0].reshape(128, 1)),
        "even_w_out": inp["even_w_out"][0],
        "odd_w_in": inp["odd_w_in"][0], "odd_w_grp": np.ascontiguousarray(inp["odd_w_grp"][0].reshape(D, 512)),
        "odd_scaleT": scaleT, "odd_w_out": inp["odd_w_out"][0],
        "xa_w_q": inp["xa_w_q"], "xa_w_k": inp["xa_w_k"], "xa_w_v": inp["xa_w_v"], "xa_w_o": inp["xa_w_o"],
        "moe_w_router": inp["moe_w_router"], "moe_b_router": inp["moe_b_router"],
        "moe_w_gu": inp["moe_w_gu"], "moe_b_guT": bguT,
        "moe_w_down": inp["moe_w_down"], "moe_b_down": inp["moe_b_down"],
        "ln_g": inp["ln_g"], "ln_b": inp["ln_b"],
    }
    maps = []
    for c in range(NCORES):
        b, r = divmod(c, 4)
        q0 = 1024 * r
        x_rel = np.zeros((SEQ, D), np.float32)
        lo = q0 - 3072
        src0 = max(lo, 0)
        x_rel[src0 - lo:] = x[b, src0:q0 + 1024]
        ptab, pinv = _pool_tables(r)
        m = dict(shared)
        m.update({
            "x_rel": x_rel, "att_bias": _att_bias(r),
            "halo_valid": np.full((128, 1), 1.0 if r > 0 else 0.0, np.float32),
            "pool_tabs": ptab, "pool_invc": pinv, "mem": inp["mem"][b],
        })
        maps.append(m)
    return maps


def kernel(**inputs):
    inp = {k: np.asarray(v, dtype=np.float32) for k, v in inputs.items()}
    p = _get_prog()
    res = run_bass_kernel_spmd(p.nc, _make_inputs(inp), core_ids=list(range(NCORES)))
    out = np.stack([np.asarray(r["y"]) for r in res.results], axis=0)
    return np.ascontiguousarray(out.reshape(2, SEQ, D)).astype(np.float32)
```

```python
import contextlib
import math
import numpy as np
import concourse.bass as bass
import concourse.mybir as mybir
from concourse.bass_utils import run_bass_kernel_spmd

F32 = mybir.dt.float32
BF16 = mybir.dt.bfloat16
AF = mybir.ActivationFunctionType
ALU = mybir.AluOpType

D = 2048
SEQ = 4096
NCORES = 8
T = 1024
NT = 8
NT9 = 9
T9 = NT9 * 128
ALPHA = (2.0 * 2) ** 0.25
LN_EPS = 1e-5
NEGBIG = -1.0e9
NE = 32
CAP = 256
SW_LIMIT = 7.0
SW_ALPHA = 1.702
RING = 3
FLEXW = 11008


class Buf:
    __slots__ = ("name", "w", "r")

    def __init__(self, name):
        self.name = name
        self.w = None
        self.r = {}


class DSem:
    __slots__ = ("key", "sem", "cnt")

    def __init__(self, key, sem):
        self.key = key
        self.sem = sem
        self.cnt = 0


class KB:
    def __init__(self, nc, es):
        self.nc = nc
        self.es = es
        self.engs = {"pe": nc.tensor, "dve": nc.vector, "act": nc.scalar,
                     "pool": nc.gpsimd, "sp": nc.sync}
        self.sems = {}
        self.cnt = {}
        self.known = {}
        for e in self.engs:
            self.sems[e] = es.enter_context(nc.semaphore("s_" + e))
            self.cnt[e] = 0
            self.known[e] = {}
        self.dsems = []

    def dsem(self, name):
        nm = "d_%s_%d" % (name, len(self.dsems))
        d = DSem(nm, self.es.enter_context(self.nc.semaphore(nm)))
        self.sems[d.key] = d.sem
        self.dsems.append(d)
        return d

    def _wait(self, eng, toks):
        kn = self.known[eng]
        need = {}
        for t in toks:
            if t is None:
                continue
            k, v = t
            if eng == "pe" and k == "pe":
                continue
            if kn.get(k, 0) < v and need.get(k, 0) < v:
                need[k] = v
        for k, v in need.items():
            self.engs[eng].wait_ge(self.sems[k], v)
            kn[k] = v

    @staticmethod
    def _deps(reads, writes):
        toks = []
        for b in reads:
            toks.append(b.w)
        for b in writes:
            toks.append(b.w)
            toks.extend(b.r.items())
        return toks

    def op(self, eng, fn, reads=(), writes=(), sig=True):
        self._wait(eng, self._deps(reads, writes))
        inst = fn(self.engs[eng])
        if not sig:
            return inst
        self.cnt[eng] += 1
        inst.then_inc(self.sems[eng], 1)
        v = self.cnt[eng]
        for b in reads:
            b.r[eng] = v
        for b in writes:
            b.w = (eng, v)
            b.r = {}
        return inst

    def dma(self, q, out, in_, dsem, reads=(), writes=()):
        self._wait(q, self._deps(reads, writes))
        inst = self.engs[q].dma_start(out=out, in_=in_)
        dsem.cnt += 16
        inst.then_inc(dsem.sem, 16)
        for b in reads:
            b.r[dsem.key] = dsem.cnt
        for b in writes:
            b.w = (dsem.key, dsem.cnt)
            b.r = {}
        return inst

    def _all(self):
        toks = [(e, self.cnt[e]) for e in self.engs if self.cnt[e] > 0]
        toks += [(d.key, d.cnt) for d in self.dsems if d.cnt > 0]
        return toks

    def barrier(self):
        toks = self._all()
        for e in self.engs:
            self._wait(e, toks)

    def finish(self):
        self._wait("sp", self._all())


class PsumPool:
    def __init__(self, kb, n=8):
        self.banks = []
        for i in range(n):
            t = kb.es.enter_context(kb.nc.psum_tensor("psb%d" % i, [128, 512], F32))
            self.banks.append((t, Buf("psb%d" % i)))
        self.pinned = set()
        self.i = 0

    def get(self, pin=False):
        n = len(self.banks)
        for _ in range(n):
            j = self.i
            self.i = (self.i + 1) % n
            if j not in self.pinned:
                if pin:
                    self.pinned.add(j)
                t, b = self.banks[j]
                return j, t, b
        raise RuntimeError("no free psum bank")

    def unpin(self, j):
        self.pinned.discard(j)


def view(base, off, n, dt=F32, pat=None, **kw):
    ap = base[:, off:off + n]
    if dt != F32:
        ap = ap.bitcast(dt)
    if pat is not None:
        ap = ap.rearrange(pat, **kw)
    return ap


class Prog:
    def __init__(self, dbg=None):
        self.dbg = dbg or ()
        self.nc = bass.Bass("TRN2", target_bir_lowering=False)
        self.inputs = {}

    def din(self, name, shape):
        ap = self.nc.dram_tensor(name, list(shape), F32, kind="ExternalInput").ap()
        self.inputs[name] = ap
        return ap

    def mark(self, name):
        if not hasattr(self, "marks"):
            self.marks = []
        self.marks.append((name, dict(self.kb.cnt)))

    def set_tokens(self, tiles, ranges):
        self.tiles = list(tiles)
        self.col0 = {t: 128 * i for i, t in enumerate(self.tiles)}
        self.ntok = 128 * len(self.tiles)
        self.ranges = list(ranges)

    def build(self):
        nc = self.nc
        with contextlib.ExitStack() as es:
            self.es = es
            kb = self.kb = KB(nc, es)
            self.pp = PsumPool(kb)
            self._declare_io()
            self._alloc()
            self._consts()
            self.L = 0
            self.set_tokens(range(NT9), [(0, 128), (128, 512), (640, 512)])
            self._mixer_even()
            self.mark("mixer0")
            self._wout_stage(self.i_wout0)
            self._layernorm(0)
            self.mark("wout_ln0")
            self._xattn()
            self._layernorm(1)
            self.mark("xattn0")
            self._moe()
            self.mark("moe0")
            self._layernorm(2)
            self.mark("ln0_2")
            self.L = 1
            self._mixer_odd()
            self.mark("mixer1")
            self.set_tokens(range(1, NT9), [(0, 512), (512, 512)])
            self._wout_stage(self.i_wout1)
            self._layernorm(0)
            self.mark("wout_ln1")
            self._xattn()
            self._layernorm(1)
            self.mark("xattn1")
            self._moe()
            self.mark("moe1")
            self._layernorm(2, make_xT=False)
            self._store_out()
            kb.finish()
        return nc

    def _declare_io(self):
        d = self.din
        self.i_xrel = d("x_rel", [SEQ, D])
        self.i_att = d("att_tabs", [5, 128, 512])
        self.i_abias = d("att_bias", [128, 768])
        self.i_hv = d("halo_valid", [128, 1])
        self.i_win0 = d("even_w_in", [D, 5120])
        self.i_lnvg = d("even_ln_v_g", [1024])
        self.i_lnvb = d("even_ln_v_b", [1024])
        self.i_wsT = d("w_sT", [128, 8 * 128])
        self.i_bs = d("even_b_s", [1024])
        self.i_lam = d("lam_vecs", [4 * 64])
        self.i_subg = d("even_subln_g", [128, 1])
        self.i_wout0 = d("even_w_out", [D, D])
        self.i_win1 = d("odd_w_in", [D, D])
        self.i_wgrp = d("odd_w_grp", [D, 512])
        self.i_scale = d("odd_scaleT", [128, 16])
        self.i_wout1 = d("odd_w_out", [D, D])
        self.i_ptab = d("pool_tabs", [128, 4 * 4 * 128])
        self.i_pinv = d("pool_invc", [128, 4 * 128])
        self.i_mem = d("mem", [256, D])
        self.i_wq = d("xa_w_q", [2, D, 512])
        self.i_wk = d("xa_w_k", [2, D, 512])
        self.i_wv = d("xa_w_v", [2, D, 512])
        self.i_wo = d("xa_w_o", [2, 512, D])
        self.i_wr = d("moe_w_router", [2, D, NE])
        self.i_br = d("moe_b_router", [2, NE])
        self.i_wgu = d("moe_w_gu", [2, NE, D, 2 * D])
        self.i_bgu = d("moe_b_guT", [2, 128, NE * 32])
        self.i_wd = d("moe_w_down", [2, NE, D, D])
        self.i_bd = d("moe_b_down", [2, NE, D])
        self.i_lng = d("ln_g", [2, 3, D])
        self.i_lnb = d("ln_b", [2, 3, D])
        self.o_y = self.nc.dram_tensor("y", [T, D], F32, kind="ExternalOutput").ap()
        self.kT_scr = self.nc.dram_tensor("kT_scr", [8, 128, SEQ], BF16, kind="Internal").ap()
        self.v_scr = self.nc.dram_tensor("v_scr", [8, 128, 32, 128], BF16, kind="Internal").ap()
        self.dbg_out = {}
        for name, shape in self.dbg:
            self.dbg_out[name] = self.nc.dram_tensor("dbg_" + name, list(shape), F32, kind="ExternalOutput").ap()

    def _alloc(self):
        nc, es, kb = self.nc, self.es, self.kb
        sb = lambda name, shape, dt: es.enter_context(nc.sbuf_tensor(name, shape, dt))
        self.XRES = sb("XRES", [128, NT9 * D], F32)
        self.CAT = sb("CAT", [128, 9216], F32)
        self.FLEX = sb("FLEX", [128, FLEXW], F32)
        self.RINGT = sb("RINGT", [128, RING * 4096], F32)
        self.ring_bufs = [Buf("ring%d" % i) for i in range(RING)]
        self.ring_ds = [kb.dsem("ring%d" % i) for i in range(RING)]
        self.ring_i = 0
        self.ident = sb("ident", [128, 128], F32)
        self.ones_bf = sb("ones_bf", [128, 128], BF16)
        self.ustrict = sb("ustrict", [128, 128], BF16)
        self.iota256 = sb("iota256", [128, 256], F32)
        self.pcol = sb("pcol", [128, 2], F32)
        self.kidx = sb("kidx", [32, 128], F32)
        self.epsc = sb("epsc", [128, 1], F32)
        self.brB = sb("brB", [128, 2 * NE], F32)
        self.lg = sb("lg", [128, NT9, NE], F32)
        self.maskt = sb("maskt", [128, NT9, NE], F32)
        self.mask_bf = sb("mask_bf", [128, NT9, NE], BF16)
        self.gate = sb("gate", [128, NT9, NE], F32)
        self.rankm = sb("rankm", [128, NT9, NE], F32)
        self.ex = sb("ex", [128, NE], F32)
        self.top8 = sb("top8", [128, 8], F32)
        self.sc = sb("sc", [128, 16], F32)
        self.stats = sb("stats", [128, 4, 6], F32)
        self.mv = sb("mv", [128, 2], F32)
        self.lamc = sb("lamc", [128, 8], F32)
        self.subg = sb("subg", [128, 1], F32)
        self.hv = sb("hv", [128, 1], F32)
        self.bgt = sb("bgt", [128, 2, 32], F32)
        self.B_const = Buf("const")
        self.B_xres = [Buf("xres%d" % i) for i in range(NT9)]
        self.B_small = Buf("small")
        self.d_misc = kb.dsem("misc")
        self.d_io = kb.dsem("io")
        self.evac_i = 0

    def xres(self, tt):
        return self.XRES[:, tt * D:(tt + 1) * D]

    def catT(self):
        return view(self.CAT, 0, self.ntok * 8, BF16, "p (c n) -> p c n", c=16)

    def xT(self):
        return view(self.FLEX, 0, self.ntok * 8, BF16, "p (c n) -> p c n", c=16)

    def wtile(self, src_ap):
        kb = self.kb
        i = self.ring_i
        self.ring_i = (i + 1) % RING
        a, b = src_ap.shape[1], src_ap.shape[2]
        dst = view(self.RINGT, i * 4096, (a * b) // 2, BF16, "p (a b) -> p a b", a=a)
        kb.dma("pool", dst, src_ap, self.ring_ds[i], writes=[self.ring_bufs[i]])
        return dst, self.ring_bufs[i]

    def ring_f32(self, n):
        i = self.ring_i
        self.ring_i = (i + 1) % RING
        return view(self.RINGT, i * 4096, n), self.ring_bufs[i], self.ring_ds[i]

    def wcols(self, w_ap, c0, n=512):
        return w_ap[:, c0:c0 + n].rearrange("(c p) n -> p c n", p=128)

    def evac(self, out, in_, reads, writes, eng=None):
        kb = self.kb
        if eng is None:
            eng = "act" if (self.evac_i % 2 == 0) else "dve"
            self.evac_i += 1
        if eng == "act":
            kb.op("act", lambda e: e.activation(out=out, in_=in_, func=AF.Copy), reads=reads, writes=writes)
        else:
            kb.op(eng, lambda e: e.tensor_copy(out=out, in_=in_), reads=reads, writes=writes)

    def dump_x(self, name):
        if name in self.dbg_out:
            for i, tt in enumerate(self.tiles):
                self.kb.dma("sp", self.dbg_out[name][i * 128:(i + 1) * 128, :], self.xres(tt), self.d_io,
                            reads=[self.B_xres[tt]])

    def _consts(self):
        kb = self.kb
        Bc = self.B_const
        ident, ones_bf, ustrict = self.ident, self.ones_bf, self.ustrict
        kb.op("pool", lambda e: e.memset(ident[:], 0.0), writes=[Bc])
        kb.op("pool", lambda e: e.affine_select(out=ident[:], in_=ident[:], pattern=[[-1, 128]],
                                                compare_op=ALU.not_equal, fill=1.0, base=0,
                                                channel_multiplier=1), reads=[Bc], writes=[Bc])
        kb.op("pool", lambda e: e.memset(ones_bf[:], 1.0), writes=[Bc])
        kb.op("pool", lambda e: e.memset(ustrict[:], 1.0), writes=[Bc])
        kb.op("pool", lambda e: e.affine_select(out=ustrict[:], in_=ustrict[:], pattern=[[1, 128]],
                                                compare_op=ALU.is_gt, fill=0.0, base=0,
                                                channel_multiplier=-1), reads=[Bc], writes=[Bc])
        kb.op("pool", lambda e: e.iota(self.iota256[:], pattern=[[1, 256]], base=0, channel_multiplier=0,
                                       allow_small_or_imprecise_dtypes=True), writes=[Bc])
        kb.op("pool", lambda e: e.iota(self.pcol[:], pattern=[[128, 2]], base=0, channel_multiplier=1,
                                       allow_small_or_imprecise_dtypes=True), writes=[Bc])
        kb.op("pool", lambda e: e.iota(self.kidx[:], pattern=[[0, 128]], base=0, channel_multiplier=1,
                                       allow_small_or_imprecise_dtypes=True), writes=[Bc])
        kb.op("pool", lambda e: e.memset(self.epsc[:], LN_EPS), writes=[Bc])
        kb.dma("sp", self.brB[:], self.i_br.rearrange("l n -> (l n)").partition_broadcast(128), self.d_misc, writes=[Bc])
        kb.dma("sp", self.hv[:], self.i_hv, self.d_misc, writes=[Bc])

    def transpose_tile(self, src_ap, src_buf, dst3, dst_buf, col0, nchunks=16):
        kb, pp = self.kb, self.pp
        for c4 in range(nchunks // 4):
            j, pt, pb = pp.get()
            for k in range(4):
                c = c4 * 4 + k
                kb.op("pe", lambda e: e.transpose(out=pt[:, k * 128:(k + 1) * 128],
                                                  in_=src_ap[:, c * 128:(c + 1) * 128],
                                                  identity=self.ident[:]),
                      reads=[src_buf, self.B_const], writes=[pb])
            self.evac(dst3[:, c4 * 4:(c4 + 1) * 4, col0:col0 + 128],
                      pt[:].rearrange("p (k n) -> p k n", k=4), [pb], [dst_buf])

    def _layernorm(self, idx, make_xT=True):
        kb = self.kb
        kb.barrier()
        gB = view(self.CAT, 0, 2048)
        bB = view(self.CAT, 2048, 2048)
        Bg = Buf("lnparams")
        kb.dma("sp", gB, self.i_lng[self.L, idx].partition_broadcast(128), self.d_misc, writes=[Bg])
        kb.dma("sp", bB, self.i_lnb[self.L, idx].partition_broadcast(128), self.d_misc, writes=[Bg])
        self.B_xT = Buf("xT")
        xT = self.xT()
        st, mv, sc = self.stats, self.mv, self.sc
        Bs = self.B_small
        for tt in self.tiles:
            x = self.xres(tt)
            Bx = self.B_xres[tt]
            for i in range(4):
                kb.op("dve", lambda e: e.bn_stats(out=st[:, i, :], in_=x[:, i * 512:(i + 1) * 512]),
                      reads=[Bx], writes=[Bs])
            kb.op("dve", lambda e: e.bn_aggr(out=mv[:], in_=st[:].rearrange("p a b -> p (a b)")),
                  reads=[Bs], writes=[Bs])
            kb.op("act", lambda e: e.activation(out=sc[:, 0:1], in_=mv[:, 1:2], func=AF.Sqrt,
                                                bias=self.epsc[:], scale=1.0),
                  reads=[Bs, self.B_const], writes=[Bs])
            kb.op("dve", lambda e: e.reciprocal(out=sc[:, 0:1], in_=sc[:, 0:1]), reads=[Bs], writes=[Bs])
            kb.op("dve", lambda e: e.tensor_scalar(out=x, in0=x, scalar1=mv[:, 0:1], scalar2=sc[:, 0:1],
                                                   op0=ALU.subtract, op1=ALU.mult),
                  reads=[Bx, Bs], writes=[Bx])
            kb.op("pool", lambda e: e.tensor_tensor(out=x, in0=x, in1=gB, op=ALU.mult),
                  reads=[Bx, Bg], writes=[Bx])
            kb.op("dve", lambda e: e.tensor_tensor(out=x, in0=x, in1=bB, op=ALU.add),
                  reads=[Bx, Bg], writes=[Bx])
            if make_xT:
                self.transpose_tile(x, Bx, xT, self.B_xT, self.col0[tt])
        self.dump_x("ln%d_%d" % (self.L, idx))
        kb.barrier()

    def _wout_stage(self, w_ap):
        kb, pp = self.kb, self.pp
        catT = self.catT()
        for cg in range(4):
            W, Bw = self.wtile(self.wcols(w_ap, cg * 512))
            for tt in self.tiles:
                c0 = self.col0[tt]
                j, pt, pb = pp.get()
                for fc in range(16):
                    kb.op("pe", lambda e: e.matmul(pt[:], lhsT=catT[:, fc, c0:c0 + 128],
                                                   rhs=W[:, fc, :], start=(fc == 0), stop=(fc == 15)), sig=(fc == 15),
                          reads=[self.B_cat, Bw], writes=[pb])
                xs = self.xres(tt)[:, cg * 512:(cg + 1) * 512]
                kb.op("dve", lambda e: e.scalar_tensor_tensor(out=xs, in0=xs, scalar=ALPHA, in1=pt[:],
                                                              op0=ALU.mult, op1=ALU.add),
                      reads=[pb, self.B_xres[tt]], writes=[self.B_xres[tt]])

    def _store_out(self):
        kb = self.kb
        for i, tt in enumerate(self.tiles):
            kb.dma("sp", self.o_y[i * 128:(i + 1) * 128, :], self.xres(tt), self.d_io,
                   reads=[self.B_xres[tt]])

    def _mixer_even(self):
        kb, pp, nc = self.kb, self.pp, self.nc
        XR, FX = self.XRES, self.FLEX
        Bc = self.B_const
        xst = [view(XR, i * 2048, 2048) for i in range(2)]
        B_xst = [Buf("xst%d" % i) for i in range(2)]
        d_xst = [kb.dsem("xst%d" % i) for i in range(2)]
        xTc = [view(XR, 4096 + i * 4096, 4096, BF16, "p (c n) -> p c n", c=16) for i in range(2)]
        B_xTc = [Buf("xTc%d" % i) for i in range(2)]
        kTst = view(XR, 12288, 2048, BF16, "p (h n) -> p h n", h=8)
        B_kTst = Buf("kTst")
        d_kT = kb.dsem("kTscr")
        vst = view(XR, 14336, 2048, BF16, "p (t h n) -> p t h n", t=4, h=8)
        B_vst = Buf("vst")
        d_v = kb.dsem("vscr")
        B_kscr = Buf("kscr")
        B_vscr = Buf("vscr")
        lnvg = view(XR, 16384, 1024)
        lnvb = view(XR, 17408, 1024)
        qT = view(FX, 0, 4608, BF16, "p (h n) -> p h n", h=8)
        B_qT = Buf("qT")
        gvt = [view(FX, 4608 + i * 1024, 1024) for i in range(2)]
        B_gvt = [Buf("gvt%d" % i) for i in range(2)]
        vln = [view(FX, 6656 + i * 512, 512, BF16) for i in range(2)]
        B_vln = [Buf("vln%d" % i) for i in range(2)]
        wsT = view(FX, 7680, 512, BF16, "p (g n) -> p g n", g=8)
        bsB = view(FX, 8192, 1024)
        wsT_f = view(FX, 9216, 1024, F32, "p (g n) -> p g n", g=8)
        ztmp = view(FX, 9216, 512)
        lamt = view(FX, 10240, 256)
        B_ztmp = Buf("ztmp")
        B_par = Buf("evenpar")
        catT = self.catT()
        self.B_cat = Buf("cat")
        B_cat = self.B_cat
        st, mv, sc = self.stats, self.mv, self.sc
        Bs = self.B_small

        kb.dma("sp", wsT_f, self.i_wsT.rearrange("p (g n) -> p g n", g=8), self.d_misc, writes=[B_par])
        kb.dma("sp", bsB, self.i_bs.partition_broadcast(128), self.d_misc, writes=[B_par])
        kb.dma("sp", lnvg, self.i_lnvg.partition_broadcast(128), self.d_misc, writes=[B_par])
        kb.dma("sp", lnvb, self.i_lnvb.partition_broadcast(128), self.d_misc, writes=[B_par])
        kb.dma("sp", lamt, self.i_lam.partition_broadcast(128), self.d_misc, writes=[B_par])
        kb.dma("sp", self.subg[:], self.i_subg, self.d_misc, writes=[B_par])
        kb.op("dve", lambda e: e.memset(wsT_f[64:128, :, 0:64], 0.0), reads=[B_par], writes=[B_par])
        kb.op("dve", lambda e: e.tensor_copy(out=wsT, in_=wsT_f), reads=[B_par], writes=[B_par])
        lambda_init = 0.8 - 0.6 * math.exp(-0.3 * 0)
        lt, lc = lamt, self.lamc
        for i in range(2):
            kb.op("dve", lambda e: e.tensor_tensor(out=lt[:, i * 128:i * 128 + 64], in0=lt[:, i * 128:i * 128 + 64],
                                                   in1=lt[:, i * 128 + 64:i * 128 + 128], op=ALU.mult),
                  reads=[B_par], writes=[B_par])
            kb.op("dve", lambda e: e.reduce_sum(out=lc[:, i:i + 1], in_=lt[:, i * 128:i * 128 + 64],
                                                axis=mybir.AxisListType.X), reads=[B_par], writes=[B_par])
            kb.op("act", lambda e: e.activation(out=lc[:, 2 + i:3 + i], in_=lc[:, i:i + 1], func=AF.Exp),
                  reads=[B_par], writes=[B_par])
        kb.op("dve", lambda e: e.tensor_tensor(out=lc[:, 4:5], in0=lc[:, 3:4], in1=lc[:, 2:3], op=ALU.subtract),
              reads=[B_par], writes=[B_par])
        kb.op("dve", lambda e: e.tensor_scalar(out=lc[:, 4:5], in0=lc[:, 4:5], scalar1=-lambda_init, scalar2=None,
                                               op0=ALU.add), reads=[B_par], writes=[B_par])
        kb.op("dve", lambda e: e.tensor_scalar(out=self.subg[:], in0=self.subg[:], scalar1=1.0 - lambda_init,
                                               scalar2=None, op0=ALU.mult), reads=[B_par], writes=[B_par])

        win = self.i_win0
        x_i = 0
        for ci in range(8):
            own = ci >= 5
            xc, Bxc = xTc[ci % 2], B_xTc[ci % 2]
            for ti in range(4):
                s = x_i % 2
                x_i += 1
                row0 = ci * 512 + ti * 128
                kb.dma("sp", xst[s], self.i_xrel[row0:row0 + 128, :], d_xst[s], writes=[B_xst[s]])
                self.transpose_tile(xst[s], B_xst[s], xc, Bxc, ti * 128)
            for j in range(2):
                W, Bw = self.wtile(self.wcols(win, 3072 + j * 512))
                for hc in range(4):
                    h = 4 * j + hc
                    jj, pt, pb = pp.get()
                    for dc in range(16):
                        kb.op("pe", lambda e: e.matmul(pt[:], lhsT=W[:, dc, hc * 128:(hc + 1) * 128],
                                                       rhs=xc[:, dc, :], start=(dc == 0), stop=(dc == 15)), sig=(dc == 15),
                              reads=[Bw, Bxc], writes=[pb])
                    self.evac(kTst[:, h, :], pt[:], [pb], [B_kTst])
            kb.dma("sp", self.kT_scr[:, :, ci * 512:(ci + 1) * 512].rearrange("h p n -> p h n"), kTst, d_kT,
                   reads=[B_kTst], writes=[B_kscr])
            for j in range(2):
                W, Bw = self.wtile(self.wcols(win, 4096 + j * 512))
                for ti in range(4):
                    jj, pt, pb = pp.get()
                    for dc in range(16):
                        kb.op("pe", lambda e: e.matmul(pt[:], lhsT=xc[:, dc, ti * 128:(ti + 1) * 128],
                                                       rhs=W[:, dc, :], start=(dc == 0), stop=(dc == 15)), sig=(dc == 15),
                              reads=[Bw, Bxc], writes=[pb])
                    self.evac(vst[:, ti, 4 * j:4 * j + 4, :], pt[:].rearrange("p (h n) -> p h n", h=4),
                              [pb], [B_vst])
            for ti in range(4):
                kb.dma("sp", self.v_scr[:, :, 4 * ci + ti, :].rearrange("h p n -> p h n"), vst[:, ti, :, :], d_v,
                       reads=[B_vst], writes=[B_vscr])
            if not own:
                continue
            tis = [3] if ci == 5 else [0, 1, 2, 3]
            cl = 384 if ci == 5 else 0
            ncol = 128 * len(tis)
            oc0 = 0 if ci == 5 else 128 + (ci - 6) * 512
            for j in range(2):
                W, Bw = self.wtile(self.wcols(win, j * 512))
                for hc in range(4):
                    g = 4 * j + hc
                    jj, pt, pb = pp.get()
                    for dc in range(16):
                        kb.op("pe", lambda e: e.matmul(pt[:, 0:ncol], lhsT=W[:, dc, hc * 128:(hc + 1) * 128],
                                                       rhs=xc[:, dc, cl:cl + ncol], start=(dc == 0), stop=(dc == 15)), sig=(dc == 15),
                              reads=[Bw, Bxc], writes=[pb])
                    kb.op("act", lambda e: e.activation(out=catT[:, g, oc0:oc0 + ncol], in_=pt[:, 0:ncol],
                                                        func=AF.Gelu), reads=[pb], writes=[B_cat])
            Wg = []
            for j in range(2):
                Wg.append(self.wtile(self.wcols(win, 1024 + j * 512)))
            for tn, ti in enumerate(tis):
                s = ti % 2
                for j in range(2):
                    W, Bw = Wg[j]
                    jj, pt, pb = pp.get()
                    for dc in range(16):
                        kb.op("pe", lambda e: e.matmul(pt[:], lhsT=xc[:, dc, ti * 128:(ti + 1) * 128],
                                                       rhs=W[:, dc, :], start=(dc == 0), stop=(dc == 15)), sig=(dc == 15),
                              reads=[Bw, Bxc], writes=[pb])
                    kb.op("act", lambda e: e.activation(out=gvt[s][:, j * 512:(j + 1) * 512], in_=pt[:],
                                                        func=AF.Gelu), reads=[pb], writes=[B_gvt[s]])
                gv = gvt[s]
                for i in range(2):
                    kb.op("dve", lambda e: e.bn_stats(out=st[:, i, :], in_=gv[:, i * 512:(i + 1) * 512]),
                          reads=[B_gvt[s]], writes=[Bs])
                kb.op("dve", lambda e: e.bn_aggr(out=mv[:], in_=st[:, 0:2, :].rearrange("p a b -> p (a b)")),
                      reads=[Bs], writes=[Bs])
                kb.op("act", lambda e: e.activation(out=sc[:, 0:1], in_=mv[:, 1:2], func=AF.Sqrt,
                                                    bias=self.epsc[:], scale=1.0), reads=[Bs, Bc], writes=[Bs])
                kb.op("dve", lambda e: e.reciprocal(out=sc[:, 0:1], in_=sc[:, 0:1]), reads=[Bs], writes=[Bs])
                kb.op("dve", lambda e: e.tensor_scalar(out=gv, in0=gv, scalar1=mv[:, 0:1], scalar2=sc[:, 0:1],
                                                       op0=ALU.subtract, op1=ALU.mult),
                      reads=[B_gvt[s], Bs], writes=[B_gvt[s]])
                kb.op("dve", lambda e: e.tensor_tensor(out=gv, in0=gv, in1=lnvg, op=ALU.mult),
                      reads=[B_gvt[s], B_par], writes=[B_gvt[s]])
                kb.op("dve", lambda e: e.tensor_tensor(out=vln[s], in0=gv, in1=lnvb, op=ALU.add),
                      reads=[B_gvt[s], B_par], writes=[B_vln[s]])
                tok0 = oc0 + tn * 128
                for half in range(2):
                    jj, pt, pb = pp.get()
                    for gi in range(4):
                        g = half * 4 + gi
                        kb.op("pe", lambda e: e.matmul(pt[:, gi * 128:(gi + 1) * 128],
                                                       lhsT=vln[s][:, g * 128:(g + 1) * 128],
                                                       rhs=wsT[:, g, :], start=True, stop=True),
                              reads=[B_vln[s], B_par], writes=[pb])
                    kb.op("dve", lambda e: e.tensor_tensor(
                        out=ztmp.rearrange("p (g n) -> p g n", g=4),
                        in0=pt[:].rearrange("p (g n) -> p g n", g=4),
                        in1=bsB.rearrange("p (g n) -> p g n", g=8)[:, half * 4:half * 4 + 4, :], op=ALU.add),
                        reads=[pb, B_par], writes=[B_ztmp])
                    ao = catT[:, half * 4:half * 4 + 4, tok0:tok0 + 128]
                    kb.op("dve", lambda e: e.tensor_tensor(out=ao, in0=ao,
                                                           in1=ztmp.rearrange("p (g n) -> p g n", g=4),
                                                           op=ALU.mult),
                          reads=[B_ztmp, B_cat], writes=[B_cat])
            for j in range(2):
                W, Bw = self.wtile(self.wcols(win, 2048 + j * 512))
                for hc in range(4):
                    h = 4 * j + hc
                    jj, pt, pb = pp.get()
                    for dc in range(16):
                        kb.op("pe", lambda e: e.matmul(pt[:, 0:ncol], lhsT=W[:, dc, hc * 128:(hc + 1) * 128],
                                                       rhs=xc[:, dc, cl:cl + ncol], start=(dc == 0), stop=(dc == 15)), sig=(dc == 15),
                              reads=[Bw, Bxc], writes=[pb])
                    self.evac(qT[:, h, oc0:oc0 + ncol], pt[:, 0:ncol], [pb], [B_qT])
        kb.barrier()
        self.mark("phaseA")

        kTh = [view(XR, i * 2048, 2048, BF16) for i in range(2)]
        B_kTh = [Buf("kTh%d" % i) for i in range(2)]
        d_kTh = [kb.dsem("kTh%d" % i) for i in range(2)]
        vh = [view(XR, 4096 + i * 2048, 2048, BF16, "p (k n) -> p k n", k=32) for i in range(2)]
        B_vh = [Buf("vh%d" % i) for i in range(2)]
        d_vh = [kb.dsem("vh%d" % i) for i in range(2)]
        tabs = view(XR, 8192, 2560, F32, "p (a n) -> p a n", a=5)
        NSB, NPT = 3, 4
        sbt = [view(XR, 10752 + i * 512, 512) for i in range(2)] + [view(XR, 14592, 512)]
        B_sbt = [Buf("sbt%d" % i) for i in range(NSB)]
        pT = [view(XR, 11776 + i * 256, 256, BF16) for i in range(3)] + [view(XR, 17152, 256, BF16)]
        B_pT = [Buf("pT%d" % i) for i in range(NPT)]
        o0 = view(XR, 12544, 512)
        o1 = view(XR, 13056, 512)
        oo = view(XR, 13568, 512)
        rL = view(XR, 14080, 512)
        sq = view(XR, 15104, 256, BF16)
        rstd = view(XR, 15360, 512)
        abias = view(XR, 16384, 768)
        Lacc = [view(XR, 17408 + i * 512, 512) for i in range(2)] + [view(FX, 10496, 512)]
        B_Lacc = [Buf("Lacc%d" % i) for i in range(3)]
        B_o = Buf("att_o")
        B_tabs = Buf("att_tabs")
        kb.dma("sp", tabs, self.i_att.rearrange("a p n -> p a n"), self.d_misc, writes=[B_tabs])
        kb.dma("sp", abias, self.i_abias, self.d_misc, writes=[B_tabs])
        scale = 64 ** -0.5
        slopes = [2.0 ** (-8.0 * (i + 1) / 8) for i in range(8)]
        qranges = [(0, 128, 24, 23), (128, 512, 28, 24), (640, 512, 32, 28)]
        DEPTH = 2
        s_i = 0
        p_i = 0
        for h in range(8):
            hb = h % 2
            kb.dma("sp", kTh[hb], self.kT_scr[h], d_kTh[hb], reads=[B_kscr], writes=[B_kTh[hb]])
            kb.dma("sp", vh[hb], self.v_scr[h], d_vh[hb], reads=[B_vscr], writes=[B_vh[hb]])
            for qi, (qc0, qn, nkb, ov0) in enumerate(qranges):
                accO = [pp.get(pin=True) for m in range(2)]
                tl = [(kbi, m) for kbi in range(nkb) for m in range(2)]
                pend = []

                def stage1(kbi, m):
                    nonlocal s_i, p_i
                    ov = kbi - ov0
                    base = tabs[:, 0, 0:qn] if ov < 0 else tabs[:, 1 + ov, 0:qn]
                    jj, pt, pb = pp.get()
                    kb.op("pe", lambda e: e.matmul(pt[:, 0:qn],
                                                   lhsT=kTh[hb][m * 64:(m + 1) * 64, kbi * 128:(kbi + 1) * 128],
                                                   rhs=qT[m * 64:(m + 1) * 64, h, qc0:qc0 + qn],
                                                   start=True, stop=True),
                          reads=[B_kTh[hb], B_qT], writes=[pb])
                    si = s_i % NSB
                    s_i += 1
                    kb.op("dve", lambda e: e.scalar_tensor_tensor(out=sbt[si][:, 0:qn], in0=base,
                                                                  scalar=slopes[h] / scale,
                                                                  in1=pt[:, 0:qn], op0=ALU.mult, op1=ALU.add),
                          reads=[pb, B_tabs], writes=[B_sbt[si]])
                    pi = p_i % NPT
                    p_i += 1
                    bidx = h * 96 + qi * 32 + kbi
                    kb.op("act", lambda e: e.activation(out=pT[pi][:, 0:qn], in_=sbt[si][:, 0:qn], func=AF.Exp,
                                                        bias=abias[:, bidx:bidx + 1], scale=scale),
                          reads=[B_sbt[si], B_tabs], writes=[B_pT[pi]])
                    if m == 1:
                        leng, la, first = "pool", 1, (kbi == 0)
                    elif kbi % 2 == 0:
                        leng, la, first = "dve", 0, (kbi == 0)
                    else:
                        leng, la, first = "pool", 2, (kbi == 1)
                    if first:
                        kb.op(leng, lambda e: e.tensor_copy(out=Lacc[la][:, 0:qn], in_=pT[pi][:, 0:qn]),
                              reads=[B_pT[pi]], writes=[B_Lacc[la]])
                    else:
                        kb.op(leng, lambda e: e.tensor_tensor(out=Lacc[la][:, 0:qn], in0=Lacc[la][:, 0:qn],
                                                              in1=pT[pi][:, 0:qn], op=ALU.add),
                              reads=[B_pT[pi], B_Lacc[la]], writes=[B_Lacc[la]])
                    return pi

                def stage2(kbi, m, pi):
                    jo, po, pbo = accO[m]
                    kb.op("pe", lambda e: e.matmul(po[:, 0:qn], lhsT=vh[hb][:, kbi, :], rhs=pT[pi][:, 0:qn],
                                                   start=(kbi == 0), stop=(kbi == nkb - 1)),
                          reads=[B_vh[hb], B_pT[pi]], writes=[pbo])

                for (kbi, m) in tl:
                    pi = stage1(kbi, m)
                    pend.append((kbi, m, pi))
                    if len(pend) > DEPTH:
                        stage2(*pend.pop(0))
                while pend:
                    stage2(*pend.pop(0))
                outs = [o0, o1]
                kb.op("dve", lambda e: e.tensor_tensor(out=Lacc[0][:, 0:qn], in0=Lacc[0][:, 0:qn], in1=Lacc[2][:, 0:qn],
                                                       op=ALU.add), reads=[B_Lacc[0], B_Lacc[2]], writes=[B_Lacc[0]])
                for m in range(2):
                    jo, po, pbo = accO[m]
                    kb.op("act", lambda e: e.activation(out=sq[:, 0:qn], in_=Lacc[m][:, 0:qn], func=AF.Copy),
                          reads=[B_Lacc[m], B_o], writes=[B_o])
                    jj, pl, pbl = pp.get()
                    kb.op("pe", lambda e: e.matmul(pl[:, 0:qn], lhsT=self.ones_bf[:], rhs=sq[:, 0:qn],
                                                   start=True, stop=True), reads=[Bc, B_o], writes=[pbl])
                    kb.op("dve", lambda e: e.tensor_scalar(out=rL[:, 0:qn], in0=pl[:, 0:qn], scalar1=1e-30,
                                                           scalar2=None, op0=ALU.max), reads=[pbl, B_o], writes=[B_o])
                    kb.op("dve", lambda e: e.reciprocal(out=rL[:, 0:qn], in_=rL[:, 0:qn]),
                          reads=[B_o], writes=[B_o])
                    kb.op("dve", lambda e: e.tensor_tensor(out=outs[m][:, 0:qn], in0=po[:, 0:qn], in1=rL[:, 0:qn],
                                                           op=ALU.mult), reads=[pbo, B_o], writes=[B_o])
                kb.op("dve", lambda e: e.scalar_tensor_tensor(out=oo[:, 0:qn], in0=o1[:, 0:qn],
                                                              scalar=self.lamc[:, 4:5], in1=o0[:, 0:qn],
                                                              op0=ALU.mult, op1=ALU.add),
                      reads=[B_o, B_par], writes=[B_o])
                kb.op("dve", lambda e: e.tensor_tensor(out=sq[:, 0:qn], in0=oo[:, 0:qn], in1=oo[:, 0:qn], op=ALU.mult),
                      reads=[B_o], writes=[B_o])
                for a in accO:
                    pp.unpin(a[0])
                jj, pt, pb = pp.get()
                kb.op("pe", lambda e: e.matmul(pt[:, 0:qn], lhsT=self.ones_bf[:], rhs=sq[:, 0:qn], start=True, stop=True),
                      reads=[Bc, B_o], writes=[pb])
                kb.op("act", lambda e: e.activation(out=rstd[:, 0:qn], in_=pt[:, 0:qn], func=AF.Sqrt, bias=self.epsc[:],
                                                    scale=1.0 / 128), reads=[pb, Bc], writes=[B_o])
                kb.op("dve", lambda e: e.reciprocal(out=rstd[:, 0:qn], in_=rstd[:, 0:qn]), reads=[B_o], writes=[B_o])
                kb.op("dve", lambda e: e.scalar_tensor_tensor(out=catT[:, 8 + h, qc0:qc0 + qn], in0=oo[:, 0:qn],
                                                              scalar=self.subg[:, 0:1], in1=rstd[:, 0:qn],
                                                              op0=ALU.mult, op1=ALU.mult),
                      reads=[B_o, B_par], writes=[B_cat])
        kb.barrier()
        for tt in self.tiles:
            kb.dma("sp", self.xres(tt), self.i_xrel[2944 + tt * 128:2944 + (tt + 1) * 128, :], self.d_io,
                   writes=[self.B_xres[tt]])
        for tt in self.tiles:
            self.B_xres[tt].w = (self.d_io.key, self.d_io.cnt)

    def _mixer_odd(self):
        kb, pp = self.kb, self.pp
        FX = self.FLEX
        self.B_cat = Buf("cat")
        B_cat = self.B_cat
        xT = self.xT()
        ptab = view(FX, 9216, 1024, BF16, "p (a j n) -> p a j n", a=4, j=4)
        invc = view(FX, 10240, 512, F32, "p (j n) -> p j n", j=4)
        scaleT = view(FX, 10752, 16)
        B_par = Buf("oddpar")
        kb.dma("pool", ptab.rearrange("p a j n -> p (a j n)"), self.i_ptab, self.d_misc, writes=[B_par])
        kb.dma("sp", invc, self.i_pinv.rearrange("p (j n) -> p j n", j=4), self.d_misc, writes=[B_par])
        kb.dma("sp", scaleT, self.i_scale, self.d_misc, writes=[B_par])
        h_bf = view(self.CAT, 0, 9216, BF16, "p (t n) -> p t n", t=NT9)
        B_h = Buf("h_bf")
        for cg in range(4):
            W, Bw = self.wtile(self.wcols(self.i_win1, cg * 512))
            for tt in range(NT9):
                jj, pt, pb = pp.get()
                for dc in range(16):
                    kb.op("pe", lambda e: e.matmul(pt[:], lhsT=xT[:, dc, tt * 128:(tt + 1) * 128], rhs=W[:, dc, :],
                                                   start=(dc == 0), stop=(dc == 15)), sig=(dc == 15),
                          reads=[Bw, self.B_xT], writes=[pb])
                self.evac(h_bf[:, tt, cg * 512:(cg + 1) * 512], pt[:], [pb], [B_h])
        kb.barrier()
        pooledT = view(FX, 0, 8192, BF16, "p (c n) -> p c n", c=16)
        B_pool = Buf("pooledT")
        wins = (2, 4, 8, 16)
        for tt in range(1, NT9):
            first = (tt == 1)
            c0 = (tt - 1) * 128
            for j in range(4):
                jj, pt, pb = pp.get()
                for k in range(4):
                    cc = 4 * j + k
                    acur = ptab[:, 0, j, :] if first else ptab[:, 1, j, :]
                    aprev = ptab[:, 2, j, :] if first else ptab[:, 3, j, :]
                    kb.op("pe", lambda e: e.matmul(pt[:, k * 128:(k + 1) * 128],
                                                   lhsT=h_bf[:, tt - 1, cc * 128:(cc + 1) * 128], rhs=aprev,
                                                   start=True, stop=False),
                          reads=[B_h, B_par], writes=[pb])
                    kb.op("pe", lambda e: e.matmul(pt[:, k * 128:(k + 1) * 128],
                                                   lhsT=h_bf[:, tt, cc * 128:(cc + 1) * 128], rhs=acur,
                                                   start=False, stop=True),
                          reads=[B_h, B_par], writes=[pb])
                dst = pooledT[:, 4 * j:4 * j + 4, c0:c0 + 128]
                src = pt[:].rearrange("p (k n) -> p k n", k=4)
                if first:
                    for k in range(4):
                        kb.op("dve", lambda e: e.tensor_tensor(out=dst[:, k, :], in0=src[:, k, :], in1=invc[:, j, :],
                                                               op=ALU.mult), reads=[pb, B_par], writes=[B_pool])
                else:
                    kb.op("act", lambda e: e.activation(out=dst, in_=src, func=AF.Copy, scale=1.0 / wins[j]),
                          reads=[pb], writes=[B_pool])
        kb.barrier()
        catT = view(self.CAT, 0, 8192, BF16, "p (c n) -> p c n", c=16)
        Wg, Bwg = self.wtile(self.i_wgrp.rearrange("(c p) n -> p c n", p=128))
        for j in range(4):
            for occ in range(4):
                for ch in range(2):
                    jj, pt, pb = pp.get()
                    for cc in range(4):
                        kb.op("pe", lambda e: e.matmul(pt[:], lhsT=Wg[:, 4 * j + cc, occ * 128:(occ + 1) * 128],
                                                       rhs=pooledT[:, 4 * j + cc, ch * 512:(ch + 1) * 512],
                                                       start=(cc == 0), stop=(cc == 3)),
                              reads=[Bwg, B_pool], writes=[pb])
                    oc = 4 * j + occ
                    kb.op("dve", lambda e: e.tensor_scalar(out=catT[:, oc, ch * 512:(ch + 1) * 512], in0=pt[:],
                                                           scalar1=scaleT[:, oc:oc + 1], scalar2=None, op0=ALU.mult),
                          reads=[pb, B_par], writes=[B_cat])
        kb.barrier()

    def _xattn(self):
        kb, pp = self.kb, self.pp
        CT = self.CAT
        Bc = self.B_const
        L = self.L
        nt = self.ntok
        xT = self.xT()
        memT = view(CT, 0, 2048, BF16, "p (c n) -> p c n", c=16)
        qTx = view(CT, 2048, 2 * nt, BF16, "p (h n) -> p h n", h=4)
        o1_ = 2048 + 2 * nt
        oTx = view(CT, o1_, 2 * nt, BF16, "p (h n) -> p h n", h=4)
        o2_ = o1_ + 2 * nt
        kTm = view(CT, o2_, 512, BF16, "p (h n) -> p h n", h=4)
        vm = view(CT, o2_ + 512, 512, BF16, "p (m n) -> p m n", m=2)
        pT = [view(CT, o2_ + 1024 + i * 256, 256, BF16) for i in range(2)]
        rL = view(CT, o2_ + 1536, 512)
        assert o2_ + 2048 <= 9216
        B_memT, B_q, B_k, B_v, B_o, B_rL = [Buf(n) for n in ("memT", "qTx", "kTm", "vm", "oTx", "rLx")]
        B_pT = [Buf("pTx%d" % i) for i in range(2)]
        for mb in range(2):
            mst, B_mst, d_mst = self.ring_f32(2048)
            kb.dma("sp", mst, self.i_mem[mb * 128:(mb + 1) * 128, :], d_mst, writes=[B_mst])
            self.transpose_tile(mst, B_mst, memT, B_memT, mb * 128)
        Wq, Bwq = self.wtile(self.wcols(self.i_wq[L], 0))
        for h in range(4):
            for (c0, n) in self.ranges:
                jj, pt, pb = pp.get()
                for dc in range(16):
                    kb.op("pe", lambda e: e.matmul(pt[:, 0:n], lhsT=Wq[:, dc, h * 128:(h + 1) * 128],
                                                   rhs=xT[:, dc, c0:c0 + n],
                                                   start=(dc == 0), stop=(dc == 15)), sig=(dc == 15),
                          reads=[Bwq, self.B_xT], writes=[pb])
                self.evac(qTx[:, h, c0:c0 + n], pt[:, 0:n], [pb], [B_q])
        Wk, Bwk = self.wtile(self.wcols(self.i_wk[L], 0))
        for h in range(4):
            jj, pt, pb = pp.get()
            for dc in range(16):
                kb.op("pe", lambda e: e.matmul(pt[:, 0:256], lhsT=Wk[:, dc, h * 128:(h + 1) * 128], rhs=memT[:, dc, :],
                                               start=(dc == 0), stop=(dc == 15)), sig=(dc == 15),
                      reads=[Bwk, B_memT], writes=[pb])
            self.evac(kTm[:, h, :], pt[:, 0:256], [pb], [B_k])
        Wv, Bwv = self.wtile(self.wcols(self.i_wv[L], 0))
        for mb in range(2):
            jj, pt, pb = pp.get()
            for dc in range(16):
                kb.op("pe", lambda e: e.matmul(pt[:], lhsT=memT[:, dc, mb * 128:(mb + 1) * 128], rhs=Wv[:, dc, :],
                                               start=(dc == 0), stop=(dc == 15)), sig=(dc == 15),
                      reads=[Bwv, B_memT], writes=[pb])
            self.evac(vm[:, mb, :], pt[:], [pb], [B_v])
        scale = 128 ** -0.5
        p_i = 0
        for h in range(4):
            for (c0, n) in self.ranges:
                jo, po, pbo = pp.get(pin=True)
                jl, pl, pbl = pp.get(pin=True)
                for mb in range(2):
                    jj, pt, pb = pp.get()
                    kb.op("pe", lambda e: e.matmul(pt[:, 0:n], lhsT=kTm[:, h, mb * 128:(mb + 1) * 128],
                                                   rhs=qTx[:, h, c0:c0 + n], start=True, stop=True),
                          reads=[B_k, B_q], writes=[pb])
                    pi = p_i % 2
                    p_i += 1
                    kb.op("act", lambda e: e.activation(out=pT[pi][:, 0:n], in_=pt[:, 0:n], func=AF.Exp, scale=scale),
                          reads=[pb], writes=[B_pT[pi]])
                    kb.op("pe", lambda e: e.matmul(po[:, 0:n], lhsT=vm[:, mb, h * 128:(h + 1) * 128], rhs=pT[pi][:, 0:n],
                                                   start=(mb == 0), stop=(mb == 1)),
                          reads=[B_v, B_pT[pi]], writes=[pbo])
                    kb.op("pe", lambda e: e.matmul(pl[:, 0:n], lhsT=self.ones_bf[:], rhs=pT[pi][:, 0:n],
                                                   start=(mb == 0), stop=(mb == 1)),
                          reads=[Bc, B_pT[pi]], writes=[pbl])
                kb.op("dve", lambda e: e.reciprocal(out=rL[:, 0:n], in_=pl[:, 0:n]), reads=[pbl], writes=[B_rL])
                kb.op("dve", lambda e: e.tensor_tensor(out=oTx[:, h, c0:c0 + n], in0=po[:, 0:n], in1=rL[:, 0:n],
                                                       op=ALU.mult), reads=[pbo, B_rL], writes=[B_o])
                pp.unpin(jo)
                pp.unpin(jl)
        Wo, Bwo = self.wtile(self.i_wo[L].rearrange("(c p) n -> p c n", p=128))
        for tt in self.tiles:
            c0 = self.col0[tt]
            for cg in range(4):
                jj, pt, pb = pp.get()
                for hc in range(4):
                    kb.op("pe", lambda e: e.matmul(pt[:], lhsT=oTx[:, hc, c0:c0 + 128],
                                                   rhs=Wo[:, hc, cg * 512:(cg + 1) * 512],
                                                   start=(hc == 0), stop=(hc == 3)),
                          reads=[Bwo, B_o], writes=[pb])
                xs = self.xres(tt)[:, cg * 512:(cg + 1) * 512]
                kb.op("dve", lambda e: e.scalar_tensor_tensor(out=xs, in0=xs, scalar=ALPHA, in1=pt[:],
                                                              op0=ALU.mult, op1=ALU.add),
                      reads=[pb, self.B_xres[tt]], writes=[self.B_xres[tt]])

    def _moe(self):
        kb, pp = self.kb, self.pp
        CT, FX = self.CAT, self.FLEX
        Bc = self.B_const
        L = self.L
        tiles = self.tiles
        ntl = len(tiles)
        nt = self.ntok
        xT = self.xT()
        x_bf = view(CT, 0, ntl * 1024, BF16, "p (t n) -> p t n", t=ntl)
        B_xbf = Buf("x_bf")
        lg, maskt, mask_bf, gate, rankm = self.lg, self.maskt, self.mask_bf, self.gate, self.rankm
        ex, top8, sc = self.ex, self.top8, self.sc
        Br = Buf("router")
        wr, B_wr = self.wtile(self.i_wr[L].rearrange("(c p) n -> p c n", p=128))
        brB = self.brB[:, L * NE:(L + 1) * NE]
        for i, tt in enumerate(tiles):
            c0 = self.col0[tt]
            self.evac(x_bf[:, i, :], self.xres(tt), [self.B_xres[tt]], [B_xbf], eng="act")
            jj, pt, pb = pp.get()
            for dc in range(16):
                kb.op("pe", lambda e: e.matmul(pt[:, 0:NE], lhsT=xT[:, dc, c0:c0 + 128], rhs=wr[:, dc, :],
                                               start=(dc == 0), stop=(dc == 15)), sig=(dc == 15),
                      reads=[self.B_xT, B_wr], writes=[pb])
            l = lg[:, i, :]
            kb.op("dve", lambda e: e.tensor_tensor(out=l, in0=pt[:, 0:NE], in1=brB, op=ALU.add),
                  reads=[pb, Bc], writes=[Br])
            kb.op("dve", lambda e: e.max(out=top8[:], in_=l), reads=[Br], writes=[Br])
            kb.op("dve", lambda e: e.tensor_scalar(out=maskt[:, i, :], in0=l, scalar1=top8[:, 3:4], scalar2=None,
                                                   op0=ALU.is_ge), reads=[Br], writes=[Br])
            if L == 0 and tt == 0:
                kb.op("dve", lambda e: e.tensor_scalar(out=maskt[:, i, :], in0=maskt[:, i, :], scalar1=self.hv[:, 0:1],
                                                       scalar2=None, op0=ALU.mult), reads=[Br, Bc], writes=[Br])
            kb.op("dve", lambda e: e.tensor_scalar(out=sc[:, 1:2], in0=top8[:, 0:1], scalar1=-1.0, scalar2=None,
                                                   op0=ALU.mult), reads=[Br], writes=[Br])
            kb.op("act", lambda e: e.activation(out=ex[:], in_=l, func=AF.Exp, bias=sc[:, 1:2], scale=1.0),
                  reads=[Br], writes=[Br])
            kb.op("dve", lambda e: e.tensor_tensor(out=ex[:], in0=ex[:], in1=maskt[:, i, :], op=ALU.mult),
                  reads=[Br], writes=[Br])
            kb.op("dve", lambda e: e.reduce_sum(out=sc[:, 2:3], in_=ex[:], axis=mybir.AxisListType.X),
                  reads=[Br], writes=[Br])
            kb.op("dve", lambda e: e.tensor_scalar(out=sc[:, 2:3], in0=sc[:, 2:3], scalar1=1e-30, scalar2=None,
                                                   op0=ALU.max), reads=[Br], writes=[Br])
            kb.op("dve", lambda e: e.reciprocal(out=sc[:, 2:3], in_=sc[:, 2:3]), reads=[Br], writes=[Br])
            kb.op("dve", lambda e: e.tensor_scalar(out=gate[:, i, :], in0=ex[:], scalar1=sc[:, 2:3], scalar2=None,
                                                   op0=ALU.mult), reads=[Br], writes=[Br])
            kb.op("dve", lambda e: e.tensor_copy(out=mask_bf[:, i, :], in_=maskt[:, i, :]), reads=[Br], writes=[Br])
        for i in range(ntl):
            jj, pt, pb = pp.get()
            for t2 in range(i):
                kb.op("pe", lambda e: e.matmul(pt[:, 0:NE], lhsT=self.ones_bf[:], rhs=mask_bf[:, t2, :],
                                               start=(t2 == 0), stop=False), reads=[Br, Bc], writes=[pb])
            kb.op("pe", lambda e: e.matmul(pt[:, 0:NE], lhsT=self.ustrict[:], rhs=mask_bf[:, i, :],
                                           start=(i == 0), stop=True), reads=[Br, Bc], writes=[pb])
            kb.op("dve", lambda e: e.scalar_tensor_tensor(out=rankm[:, i, :], in0=pt[:, 0:NE], scalar=1.0,
                                                          in1=maskt[:, i, :], op0=ALU.add, op1=ALU.mult),
                  reads=[pb, Br], writes=[Br])
            kb.op("dve", lambda e: e.tensor_scalar(out=rankm[:, i, :], in0=rankm[:, i, :], scalar1=-1.0,
                                                   scalar2=None, op0=ALU.add), reads=[Br], writes=[Br])
        for tt in tiles:
            kb.op("pool", lambda e: e.tensor_scalar(out=self.xres(tt), in0=self.xres(tt), scalar1=ALPHA,
                                                    scalar2=None, op0=ALU.mult),
                  reads=[self.B_xres[tt], B_xbf], writes=[self.B_xres[tt]])
        kb.barrier()
        xselT = view(FX, 0, 2048, BF16, "p (c n) -> p c n", c=16)
        actT = view(FX, 2048, 2048, BF16, "p (c n) -> p c n", c=16)
        yeF = view(FX, 4096, 2048, BF16, "p (s n) -> p s n", s=2)
        S = view(FX, 6144, ntl * 128, BF16, "p (t n) -> p t n", t=ntl)
        ST = view(FX, 7296, nt, BF16, "p (s n) -> p s n", s=2)
        gl = view(FX, 8448, 512, BF16, "p (k n) -> p k n", k=4)
        rankmT = self.FLEX[0:32, 8960:8960 + nt]
        esel = self.FLEX[0:32, 10112:10240]
        gS = [view(FX, 10240 + i * 256, 256) for i in range(2)]
        sS = view(FX, 10752, 256)
        assert 10752 + 256 <= FLEXW and 8960 + nt <= 10112
        B_xsel, B_act, B_S, B_ST, B_gl, B_rT, B_gT, B_esel, B_sS = [
            Buf(n) for n in ("xselT", "actT", "S", "ST", "gl", "rankmT", "gateT", "esel", "sS")]
        B_ye = [Buf("ye%d" % i) for i in range(4)]
        B_gS = [Buf("gS%d" % i) for i in range(2)]
        B_bg = [Buf("bg%d" % i) for i in range(2)]
        if not hasattr(self, "d_bg"):
            self.d_bg = [kb.dsem("bg%d" % i) for i in range(2)]
        d_bg = self.d_bg

        def transpose_table(src, dst, Bd):
            for b0 in range(0, ntl, 4):
                nb = min(4, ntl - b0)
                jj, pt, pb = pp.get()
                for k in range(nb):
                    kb.op("pe", lambda e: e.transpose(out=pt[0:32, k * 128:(k + 1) * 128], in_=src[:, b0 + k, :],
                                                      identity=self.ident[:]), reads=[Br, Bc], writes=[pb])
                self.evac(dst[:, b0 * 128:(b0 + nb) * 128], pt[0:32, 0:nb * 128], [pb], [Bd], eng="dve")

        transpose_table(rankm, rankmT, B_rT)
        g_i = 0

        def build_S(ei):
            for i in range(ntl):
                kb.op("dve", lambda e: e.tensor_scalar(out=S[:, i, :], in0=self.iota256[:],
                                                       scalar1=rankm[:, i, ei:ei + 1], scalar2=None,
                                                       op0=ALU.is_equal), reads=[Br, Bc], writes=[B_S])

        def gather_part(d2s):
            for d2 in d2s:
                jj, pt, pb = pp.get()
                for k in range(2):
                    dc = 2 * d2 + k
                    for i in range(ntl):
                        kb.op("pe", lambda e: e.matmul(pt[:, k * 256:(k + 1) * 256],
                                                       lhsT=x_bf[:, i, dc * 128:(dc + 1) * 128], rhs=S[:, i, :],
                                                       start=(i == 0), stop=(i == ntl - 1)), sig=(i == ntl - 1),
                              reads=[B_xbf, B_S], writes=[pb])
                self.evac(xselT[:, 2 * d2:2 * d2 + 2, :], pt[:].rearrange("p (k n) -> p k n", k=2), [pb], [B_xsel])

        def build_ST(ei):
            kb.op("dve", lambda e: e.tensor_scalar(out=esel, in0=self.kidx[:], scalar1=float(ei), scalar2=None,
                                                   op0=ALU.is_equal), reads=[Bc], writes=[B_esel])
            for (c0, n) in self.ranges:
                jj, pt, pb = pp.get()
                kb.op("pe", lambda e: e.matmul(pt[:, 0:n], lhsT=esel, rhs=rankmT[:, c0:c0 + n],
                                               start=True, stop=True), reads=[B_esel, B_rT], writes=[pb])
                for sh in range(2):
                    kb.op("dve", lambda e: e.tensor_scalar(out=ST[:, sh, c0:c0 + n], in0=pt[:, 0:n],
                                                           scalar1=self.pcol[:, sh:sh + 1], scalar2=None,
                                                           op0=ALU.is_equal), reads=[pb, Bc], writes=[B_ST])

        def scatter(ep, cg):
            for i, tt in enumerate(tiles):
                c0 = self.col0[tt]
                jj, pt, pb = pp.get()
                for sh in range(2):
                    kb.op("pe", lambda e: e.matmul(pt[:], lhsT=ST[:, sh, c0:c0 + 128],
                                                   rhs=yeF[:, sh, cg * 512:(cg + 1) * 512],
                                                   start=(sh == 0), stop=(sh == 1)),
                          reads=[B_ST, B_ye[cg]], writes=[pb])
                xs = self.xres(tt)[:, cg * 512:(cg + 1) * 512]
                kb.op("dve", lambda e: e.scalar_tensor_tensor(out=xs, in0=pt[:], scalar=gate[:, i, ep:ep + 1],
                                                              in1=xs, op0=ALU.mult, op1=ALU.add),
                      reads=[pb, Br, self.B_xres[tt]], writes=[self.B_xres[tt]])

        build_S(0)
        gather_part(range(8))
        for ei in range(NE):
            eb = ei % 2
            bg = self.bgt[:, eb, :]
            kb.dma("sp", bg, self.i_bgu[L, :, ei * 32:(ei + 1) * 32], d_bg[eb], writes=[B_bg[eb]])
            for j in range(4):
                Wg_, Bwg = self.wtile(self.wcols(self.i_wgu[L, ei], j * 512))
                Wl_, Bwl = self.wtile(self.wcols(self.i_wgu[L, ei], 2048 + j * 512))
                for k2 in range(2):
                    jj, pt, pb = pp.get()
                    for k1 in range(2):
                        k = 2 * k2 + k1
                        for dc in range(16):
                            kb.op("pe", lambda e: e.matmul(pt[:, k1 * 256:(k1 + 1) * 256],
                                                           lhsT=Wg_[:, dc, k * 128:(k + 1) * 128], rhs=xselT[:, dc, :],
                                                           start=(dc == 0), stop=(dc == 15)), sig=(dc == 15),
                                  reads=[Bwg, B_xsel], writes=[pb])
                    for k1 in range(2):
                        k = 2 * k2 + k1
                        c = 4 * j + k
                        gi = g_i % 2
                        g_i += 1
                        kb.op("dve", lambda e: e.tensor_scalar(out=gS[gi], in0=pt[:, k1 * 256:(k1 + 1) * 256],
                                                               scalar1=bg[:, c:c + 1], scalar2=SW_LIMIT,
                                                               op0=ALU.add, op1=ALU.min),
                              reads=[pb, B_bg[eb]], writes=[B_gS[gi]])
                        kb.op("act", lambda e: e.activation(out=sS, in_=gS[gi], func=AF.Sigmoid, scale=SW_ALPHA),
                              reads=[B_gS[gi]], writes=[B_sS])
                        kb.op("dve", lambda e: e.tensor_tensor(out=gl[:, k, :], in0=gS[gi], in1=sS, op=ALU.mult),
                              reads=[B_gS[gi], B_sS], writes=[B_gl])
                for k2 in range(2):
                    jj, pt, pb = pp.get()
                    for k1 in range(2):
                        k = 2 * k2 + k1
                        for dc in range(16):
                            kb.op("pe", lambda e: e.matmul(pt[:, k1 * 256:(k1 + 1) * 256],
                                                           lhsT=Wl_[:, dc, k * 128:(k + 1) * 128], rhs=xselT[:, dc, :],
                                                           start=(dc == 0), stop=(dc == 15)), sig=(dc == 15),
                                  reads=[Bwl, B_xsel], writes=[pb])
                    for k1 in range(2):
                        k = 2 * k2 + k1
                        c = 4 * j + k
                        gi = g_i % 2
                        g_i += 1
                        lS = gS[gi]
                        kb.op("dve", lambda e: e.tensor_scalar(out=lS, in0=pt[:, k1 * 256:(k1 + 1) * 256],
                                                               scalar1=bg[:, 16 + c:17 + c], scalar2=SW_LIMIT,
                                                               op0=ALU.add, op1=ALU.min),
                              reads=[pb, B_bg[eb]], writes=[B_gS[gi]])
                        kb.op("dve", lambda e: e.tensor_scalar(out=lS, in0=lS, scalar1=-SW_LIMIT, scalar2=1.0,
                                                               op0=ALU.max, op1=ALU.add),
                              reads=[B_gS[gi]], writes=[B_gS[gi]])
                        kb.op("dve", lambda e: e.tensor_tensor(out=actT[:, c, :], in0=gl[:, k, :], in1=lS,
                                                               op=ALU.mult),
                              reads=[B_gl, B_gS[gi]], writes=[B_act])
                if ei > 0:
                    scatter(ei - 1, j)
            build_ST(ei)
            nxt = ei + 1 < NE
            if nxt:
                build_S(ei + 1)
            for cg in range(4):
                Wd_, Bwd = self.wtile(self.wcols(self.i_wd[L, ei], cg * 512))
                for sh in range(2):
                    jj, pt, pb = pp.get()
                    for fc in range(16):
                        kb.op("pe", lambda e: e.matmul(pt[:], lhsT=actT[:, fc, sh * 128:(sh + 1) * 128], rhs=Wd_[:, fc, :],
                                                       start=(fc == 0), stop=(fc == 15)), sig=(fc == 15),
                              reads=[Bwd, B_act], writes=[pb])
                    self.evac(yeF[:, sh, cg * 512:(cg + 1) * 512], pt[:], [pb], [B_ye[cg]], eng="act")
                if nxt:
                    gather_part([2 * cg, 2 * cg + 1])
        for cg in range(4):
            scatter(NE - 1, cg)
        kb.barrier()
        gateT = self.FLEX[0:32, 0:nt]
        bd = self.FLEX[0:32, 1152:1152 + 2048]
        B_bd = Buf("bd")
        kb.dma("sp", bd, self.i_bd[L], self.d_misc, writes=[B_bd])
        transpose_table(gate, gateT, B_gT)
        for tt in tiles:
            c0 = self.col0[tt]
            for cg in range(4):
                jj, pt, pb = pp.get()
                kb.op("pe", lambda e: e.matmul(pt[:], lhsT=gateT[:, c0:c0 + 128],
                                               rhs=bd[:, cg * 512:(cg + 1) * 512], start=True, stop=True),
                      reads=[B_gT, B_bd], writes=[pb])
                xs = self.xres(tt)[:, cg * 512:(cg + 1) * 512]
                kb.op("dve", lambda e: e.tensor_tensor(out=xs, in0=xs, in1=pt[:], op=ALU.add),
                      reads=[pb, self.B_xres[tt]], writes=[self.B_xres[tt]])


_PROG = {}


def _get_prog(dbg=None):
    key = tuple(dbg or ())
    if key not in _PROG:
        p = Prog(dbg)
        p.build()
        _PROG[key] = p
    return _PROG[key]


def _att_tables():
    ki = np.arange(128, dtype=np.float64)[:, None]
    qi = np.arange(512, dtype=np.float64)[None, :]
    tabs = np.zeros((5, 128, 512), np.float32)
    tabs[0] = ki - qi
    for o in range(4):
        kpos = 128 * o + ki
        allowed = (64 * np.floor(kpos / 64)) <= qi
        tabs[1 + o] = np.where(allowed, -np.abs(qi - kpos), NEGBIG)
    return tabs


def _att_bias(r):
    slopes = [2.0 ** (-8.0 * (i + 1) / 8) for i in range(8)]
    qranges = [(2944, 24, 23), (3072, 28, 24), (3584, 32, 28)]
    b = np.zeros((8, 3, 32), np.float32)
    first_valid = (3072 - 1024 * r) // 128
    for h in range(8):
        for qi, (qs, nkb, ov0) in enumerate(qranges):
            for kb_ in range(32):
                if kb_ >= nkb:
                    v = NEGBIG
                elif kb_ >= ov0:
                    v = 0.0
                else:
                    v = -slopes[h] * (qs - 128 * kb_)
                if kb_ < first_valid:
                    v = NEGBIG
                b[h, qi, kb_] = v
    return np.ascontiguousarray(np.broadcast_to(b.reshape(1, 768), (128, 768)))


def _pool_tables(r):
    wins = (2, 4, 8, 16)
    s = np.arange(128)[:, None]
    t = np.arange(128)[None, :]
    tabs = np.zeros((128, 4, 4, 128), np.float32)
    invc = np.zeros((128, 4, 128), np.float32)
    for j, w in enumerate(wins):
        band = ((s <= t) & (s > t - w)).astype(np.float32)
        cnt_first = np.minimum(np.arange(128) + 1, w).astype(np.float32) if r == 0 else np.full(128, float(w), np.float32)
        prev = ((s - 128) > (t - w)).astype(np.float32)
        tabs[:, 0, j, :] = band - np.eye(128, dtype=np.float32) * cnt_first[None, :]
        tabs[:, 1, j, :] = band - np.eye(128, dtype=np.float32) * float(w)
        tabs[:, 2, j, :] = prev if r > 0 else 0.0
        tabs[:, 3, j, :] = prev
        invc[:, j, :] = (1.0 / cnt_first)[None, :]
    return tabs.reshape(128, -1), invc.reshape(128, -1)


def _make_inputs(inp):
    x = inp["x"]
    tabs = _att_tables()
    w_sT = np.ascontiguousarray(inp["even_w_s"][0].transpose(2, 0, 1).reshape(128, 8 * 128))
    lam = np.ascontiguousarray(np.concatenate([inp["even_lam_q1"][0], inp["even_lam_k1"][0],
                                               inp["even_lam_q2"][0], inp["even_lam_k2"][0]]))
    bgu = inp["moe_b_gu"]
    bguT = np.ascontiguousarray(bgu.reshape(2, NE, 32, 128).transpose(0, 3, 1, 2).reshape(2, 128, NE * 32))
    scaleT = np.ascontiguousarray(inp["odd_scale"][0].reshape(16, 128).T)
    shared = {
        "att_tabs": tabs,
        "even_w_in": inp["even_w_in"][0], "even_ln_v_g": inp["even_ln_v_g"][0], "even_ln_v_b": inp["even_ln_v_b"][0],
        "w_sT": w_sT, "even_b_s": np.ascontiguousarray(inp["even_b_s"][0].reshape(-1)),
        "lam_vecs": lam, "even_subln_g": np.ascontiguousarray(inp["even_subln_g"][0].reshape(128, 1)),
        "even_w_out": inp["even_w_out"][0],
        "odd_w_in": inp["odd_w_in"][0], "odd_w_grp": np.ascontiguousarray(inp["odd_w_grp"][0].reshape(D, 512)),
        "odd_scaleT": scaleT, "odd_w_out": inp["odd_w_out"][0],
        "xa_w_q": inp["xa_w_q"], "xa_w_k": inp["xa_w_k"], "xa_w_v": inp["xa_w_v"], "xa_w_o": inp["xa_w_o"],
        "moe_w_router": inp["moe_w_router"], "moe_b_router": inp["moe_b_router"],
        "moe_w_gu": inp["moe_w_gu"], "moe_b_guT": bguT,
        "moe_w_down": inp["moe_w_down"], "moe_b_down": inp["moe_b_down"],
        "ln_g": inp["ln_g"], "ln_b": inp["ln_b"],
    }
    maps = []
    for c in range(NCORES):
        b, r = divmod(c, 4)
        q0 = 1024 * r
        x_rel = np.zeros((SEQ, D), np.float32)
        lo = q0 - 3072
        src0 = max(lo, 0)
        x_rel[src0 - lo:] = x[b, src0:q0 + 1024]
        ptab, pinv = _pool_tables(r)
        m = dict(shared)
        m.update({
            "x_rel": x_rel, "att_bias": _att_bias(r),
            "halo_valid": np.full((128, 1), 1.0 if r > 0 else 0.0, np.float32),
            "pool_tabs": ptab, "pool_invc": pinv, "mem": inp["mem"][b],
        })
        maps.append(m)
    return maps


def kernel(**inputs):
    inp = {k: np.asarray(v, dtype=np.float32) for k, v in inputs.items()}
    p = _get_prog()
    res = run_bass_kernel_spmd(p.nc, _make_inputs(inp), core_ids=list(range(NCORES)))
    out = np.stack([np.asarray(r["y"]) for r in res.results], axis=0)
    return np.ascontiguousarray(out.reshape(2, SEQ, D)).astype(np.float32)
```
